# Optimizing a Trainium2 kernel written in Bass

```python
import math
import jax, jax.numpy as jnp
from jax import lax
import numpy as np

D_MODEL = 1024
BATCH = 16
SEQ = 2048
DEPTH = 4

GRID_W = 64
CTX_LEN = 256
N_BRANCH = 4
BRANCH_W = D_MODEL // 2
RNN_W = BRANCH_W
RNN_BLOCK = 64
RNN_BLOCKS = RNN_W // RNN_BLOCK
RG_C = 8.0
CONV_W = 4
ATT_DIM = 64
ATT_HEADS = BRANCH_W // (2 * ATT_DIM)
ATT_QK_W = ATT_HEADS * 2 * ATT_DIM
ATT_V_W = ATT_HEADS * 2 * ATT_DIM
ROPE_F = ATT_DIM // 4
ROPE_BASE = 10000.0
Q_BLOCK = 128
POOL_W = BRANCH_W
POOL_WINDOWS = (2, 4, 8, 16)
POOL_GROUPS = len(POOL_WINDOWS)
POOL_GW = POOL_W // POOL_GROUPS
SSM_W = BRANCH_W
SSM_P = 64
SSM_HEADS = SSM_W // SSM_P
SSM_G = 2
SSM_N = 64
SSM_XBC_W = SSM_W + 2 * SSM_G * SSM_N
SSM_CHUNK = 128
N_EXPERTS = 32
TOP_K = 4
D_EXPERT = D_MODEL
SWIGLU_LIMIT = 7.0
SWIGLU_ALPHA = 1.702
EXPERT_BLOCK = 256
DN_ALPHA = (2 * DEPTH) ** 0.25
DN_BETA = (8 * DEPTH) ** -0.25
IN_WIDTHS = (RNN_W, RNN_W, ATT_QK_W, ATT_QK_W, ATT_V_W, POOL_W, SSM_W, SSM_XBC_W, 2 * SSM_HEADS, N_BRANCH * D_MODEL)
IN_W = sum(IN_WIDTHS)
IN_SPLITS = tuple(int(v) for v in np.cumsum(IN_WIDTHS)[:-1])

kernel_name = 'hybrid_diffusion_gated_mixers_moe'


def _normalize(x, eps=1e-6):
    xf = x.astype(jnp.float32)
    mu = jnp.mean(xf, -1, keepdims=True)
    var = jnp.mean(jnp.square(xf - mu), -1, keepdims=True)
    return ((xf - mu) * lax.rsqrt(var + eps)).astype(x.dtype)


def layer_norm(x, g, b):
    return _normalize(x, 1e-5) * g + b


def modulate(x, shift, scale):
    return _normalize(x) * (1.0 + scale) + shift


def centred_dwconv(u, w, b):
    length = u.shape[1]
    left = CONV_W // 2
    up = jnp.pad(u, ((0, 0), (left, CONV_W - 1 - left), (0, 0)))
    out = b + up[:, 0:length] * w[0]
    for k in range(1, CONV_W):
        out = out + up[:, k:k + length] * w[k]
    return out


def linear_recurrence(a, b, h0, reverse):
    def combine(left, right):
        a_l, b_l = left
        a_r, b_r = right
        return a_l * a_r, a_r * b_l + b_r
    a_cum, h = lax.associative_scan(combine, (a, b), reverse=reverse, axis=1)
    h = h + a_cum * h0[:, None]
    final = h[:, 0] if reverse else h[:, -1]
    return h, final


def rglru_coeffs(u, wa, ba, wx, bx, lam):
    bsz, length, width = u.shape
    ub = u.reshape(bsz, length, RNN_BLOCKS, RNN_BLOCK)
    r = jax.nn.sigmoid(jnp.einsum('blhi,hij->blhj', ub, wa).reshape(bsz, length, width) + ba)
    i = jax.nn.sigmoid(jnp.einsum('blhi,hij->blhj', ub, wx).reshape(bsz, length, width) + bx)
    log_a = -RG_C * r * jax.nn.softplus(-lam)
    a = jnp.exp(log_a)
    mult = jnp.sqrt(-jnp.expm1(2.0 * log_a))
    return a, mult * (i * u)


def rglru_branch(u_c, u_l, gate_c, gate_l, conv_w, conv_b, wa, ba, wx, bx, lam, need_ctx):
    dtype = u_l.dtype
    u_c = centred_dwconv(u_c, conv_w, conv_b).astype(jnp.float32)
    u_l = centred_dwconv(u_l, conv_w, conv_b).astype(jnp.float32)
    h0 = jnp.zeros((u_c.shape[0], RNN_W), jnp.float32)
    y_c = jnp.zeros_like(u_c)
    y_l = jnp.zeros_like(u_l)
    for d in range(2):
        rev = d == 1
        a_c, b_c = rglru_coeffs(u_c, wa[d], ba[d], wx[d], bx[d], lam[d])
        a_l, b_l = rglru_coeffs(u_l, wa[d], ba[d], wx[d], bx[d], lam[d])
        h_c, h_fin = linear_recurrence(a_c, b_c, h0, rev)
        h_l, _ = linear_recurrence(a_l, b_l, h_fin, rev)
        y_c = y_c + h_c
        y_l = y_l + h_l
    out_l = (jax.nn.gelu(gate_l.astype(jnp.float32)) * y_l).astype(dtype)
    out_c = (jax.nn.gelu(gate_c.astype(jnp.float32)) * y_c).astype(dtype) if need_ctx else None
    return out_l, out_c


def axial_rope_tables(n_lat):
    rows = n_lat // GRID_W
    row = jnp.repeat(jnp.arange(rows, dtype=jnp.float32), GRID_W)
    col = jnp.tile(jnp.arange(GRID_W, dtype=jnp.float32), rows)
    inv = ROPE_BASE ** (-jnp.arange(ROPE_F, dtype=jnp.float32) / ROPE_F)
    ang = jnp.stack([row[:, None] * inv, col[:, None] * inv], axis=1)
    return jnp.cos(ang), jnp.sin(ang)


def rope_2d(x, cos, sin):
    xs = x.astype(jnp.float32).reshape(*x.shape[:-1], 2, 2, ROPE_F)
    x1, x2 = xs[..., 0, :], xs[..., 1, :]
    cs = cos[None, :, None, None]
    sn = sin[None, :, None, None]
    out = jnp.stack([x1 * cs - x2 * sn, x2 * cs + x1 * sn], axis=-2)
    return out.reshape(x.shape).astype(x.dtype)


def diff_softmax_attend(q, k, v, lam):
    s = jnp.einsum('bqhcd,bkhcd->bhcqk', q, k).astype(jnp.float32) * (ATT_DIM ** -0.5)
    p = jax.nn.softmax(s, axis=-1)
    w = p[:, :, 0] - lam * p[:, :, 1]
    return jnp.einsum('bhqk,bkhe->bqhe', w.astype(v.dtype), v)


def diff_head_norm(o, subln_w, lam_init):
    bsz, length = o.shape[:2]
    of = o.astype(jnp.float32)
    of = of * lax.rsqrt(jnp.mean(jnp.square(of), -1, keepdims=True) + 1e-5) * subln_w * (1.0 - lam_init)
    return of.reshape(bsz, length, ATT_V_W).astype(o.dtype)


def diff_attention_branch(q_c, k_c, v_c, q_l, k_l, v_l, lam_vecs, subln_w, layer_idx, cos, sin, need_ctx):
    bsz, n_lat = q_l.shape[:2]
    n_ctx = k_c.shape[1]
    heads = lambda t: t.reshape(t.shape[0], t.shape[1], ATT_HEADS, 2, ATT_DIM)
    q_l = rope_2d(heads(q_l), cos, sin)
    k_l = rope_2d(heads(k_l), cos, sin)
    k_c = heads(k_c)
    v_l = v_l.reshape(bsz, n_lat, ATT_HEADS, 2 * ATT_DIM)
    v_c = v_c.reshape(bsz, n_ctx, ATT_HEADS, 2 * ATT_DIM)
    lam_init = 0.8 - 0.6 * math.exp(-0.3 * layer_idx)
    lv = lam_vecs.astype(jnp.float32)
    lam = jnp.exp(jnp.sum(lv[0] * lv[1])) - jnp.exp(jnp.sum(lv[2] * lv[3])) + lam_init
    k_all = jnp.concatenate([k_c, k_l], axis=1)
    v_all = jnp.concatenate([v_c, v_l], axis=1)
    n_blk = n_lat // Q_BLOCK
    qb = jnp.swapaxes(q_l.reshape(bsz, n_blk, Q_BLOCK, ATT_HEADS, 2, ATT_DIM), 0, 1)
    ob = lax.map(lambda qq: diff_softmax_attend(qq, k_all, v_all, lam), qb)
    o_l = jnp.swapaxes(ob, 0, 1).reshape(bsz, n_lat, ATT_HEADS, 2 * ATT_DIM)
    out_l = diff_head_norm(o_l, subln_w, lam_init)
    out_c = None
    if need_ctx:
        out_c = diff_head_norm(diff_softmax_attend(heads(q_c), k_c, v_c, lam), subln_w, lam_init)
    return out_l, out_c


def multiscale_pool(u, w, b, scale):
    bsz, length, _ = u.shape
    ug = u.astype(jnp.float32).reshape(bsz, length, POOL_GROUPS, POOL_GW)
    csum = jnp.pad(jnp.cumsum(ug, axis=1), ((0, 0), (1, 0), (0, 0), (0, 0)))
    t = jnp.arange(length)
    pooled = []
    for gi, win in enumerate(POOL_WINDOWS):
        lo = jnp.clip(t - win // 2, 0, length - 1)
        hi = jnp.clip(t + win - 1 - win // 2, 0, length - 1)
        cnt = (hi - lo + 1).astype(jnp.float32)[None, :, None]
        sg = csum[:, :, gi]
        pooled.append((sg[:, hi + 1] - sg[:, lo]) / cnt)
    mix = jnp.stack(pooled, axis=2) - ug
    y = jnp.einsum('blgc,gce->blge', mix, w).reshape(bsz, length, POOL_W) + b
    return (y * scale).astype(u.dtype)


def segsum(a):
    t = a.shape[-1]
    cs = jnp.cumsum(a, axis=-1)
    diff = cs[..., :, None] - cs[..., None, :]
    mask = jnp.tril(jnp.ones((t, t), dtype=bool))
    return jnp.where(mask, diff, -jnp.inf)


def ssd_scan(xdt, adt, bm, cm, h0):
    b, l, h, p = xdt.shape
    g, n = bm.shape[2], bm.shape[3]
    k = h // g
    c = l // SSM_CHUNK
    X = xdt.reshape(b, c, SSM_CHUNK, g, k, p)
    A = adt.reshape(b, c, SSM_CHUNK, g, k).transpose(0, 3, 4, 1, 2)
    Bc = bm.reshape(b, c, SSM_CHUNK, g, n)
    Cc = cm.reshape(b, c, SSM_CHUNK, g, n)
    a_cs = jnp.cumsum(A, axis=-1)
    lmat = jnp.exp(segsum(A))
    cb = jnp.einsum('bclgn,bcsgn->bgcls', Cc, Bc)
    y_diag = jnp.einsum('bgcls,bgkcls,bcsgkp->bclgkp', cb, lmat, X)
    decay_states = jnp.exp(a_cs[..., -1:] - a_cs)
    states = jnp.einsum('bclgn,bgkcl,bclgkp->bcgkpn', Bc, decay_states, X)
    states = jnp.concatenate([h0.reshape(b, g, k, p, n)[:, None], states], axis=1)
    chunk_a = jnp.pad(a_cs[..., -1], ((0, 0), (0, 0), (0, 0), (1, 0)))
    decay_chunk = jnp.exp(segsum(chunk_a))
    new_states = jnp.einsum('bgkzc,bcgkpn->bzgkpn', decay_chunk, states)
    prev_states, final = new_states[:, :-1], new_states[:, -1]
    y_off = jnp.einsum('bclgn,bcgkpn,bgkcl->bclgkp', Cc, prev_states, jnp.exp(a_cs))
    y = (y_diag + y_off).reshape(b, l, h, p)
    return y, final.reshape(b, h, p, n)


def ssd_prep(xbc, conv_w, conv_b):
    b, l, _ = xbc.shape
    u = jax.nn.silu(centred_dwconv(xbc, conv_w, conv_b).astype(jnp.float32))
    xs, bm, cm = jnp.split(u, (SSM_W, SSM_W + SSM_G * SSM_N), axis=-1)
    return (xs.reshape(b, l, SSM_HEADS, SSM_P), bm.reshape(b, l, SSM_G, SSM_N), cm.reshape(b, l, SSM_G, SSM_N))


def ssd_direction(xs, bm, cm, dt_raw, dt_bias, a_log, d_skip, h0, reverse):
    dt = jax.nn.softplus(dt_raw.astype(jnp.float32) + dt_bias)
    a = -jnp.exp(a_log.astype(jnp.float32))
    xdt = xs * dt[..., None]
    adt = dt * a
    if reverse:
        xdt, adt, bm, cm = (jnp.flip(t, axis=1) for t in (xdt, adt, bm, cm))
    y, h_fin = ssd_scan(xdt, adt, bm, cm, h0)
    if reverse:
        y = jnp.flip(y, axis=1)
    return y + d_skip[:, None] * xs, h_fin


def gated_rmsnorm(y, z, w):
    b, l = y.shape[:2]
    g = (y.reshape(b, l, SSM_W) * jax.nn.silu(z.astype(jnp.float32))).reshape(b, l, SSM_G, SSM_W // SSM_G)
    g = g * lax.rsqrt(jnp.mean(jnp.square(g), -1, keepdims=True) + 1e-5)
    return g.reshape(b, l, SSM_W) * w


def ssd_branch(z_c, xbc_c, dt_c, z_l, xbc_l, dt_l, conv_w, conv_b, dt_bias, a_log, d_skip, norm_w, need_ctx):
    dtype = z_l.dtype
    xs_c, b_c, c_c = ssd_prep(xbc_c, conv_w, conv_b)
    xs_l, b_l, c_l = ssd_prep(xbc_l, conv_w, conv_b)
    h0 = jnp.zeros((xs_c.shape[0], SSM_HEADS, SSM_P, SSM_N), jnp.float32)
    y_c = jnp.zeros_like(xs_c)
    y_l = jnp.zeros_like(xs_l)
    for d in range(2):
        sl = slice(d * SSM_HEADS, (d + 1) * SSM_HEADS)
        yc_d, h_fin = ssd_direction(xs_c, b_c, c_c, dt_c[..., sl], dt_bias[d], a_log[d], d_skip[d], h0, d == 1)
        yl_d, _ = ssd_direction(xs_l, b_l, c_l, dt_l[..., sl], dt_bias[d], a_log[d], d_skip[d], h_fin, d == 1)
        y_c = y_c + yc_d
        y_l = y_l + yl_d
    out_l = gated_rmsnorm(y_l, z_l, norm_w).astype(dtype)
    out_c = gated_rmsnorm(y_c, z_c, norm_w).astype(dtype) if need_ctx else None
    return out_l, out_c


def merge_branches(ys, gates_raw, w_branch, w_out):
    b, l, _ = gates_raw.shape
    g = jax.nn.sigmoid(gates_raw.astype(jnp.float32)).reshape(b, l, N_BRANCH, D_MODEL).astype(gates_raw.dtype)
    m = g[:, :, 0] * (ys[0] @ w_branch[0])
    for i in range(1, N_BRANCH):
        m = m + g[:, :, i] * (ys[i] @ w_branch[i])
    return m @ w_out


def moe_ffn(h, router_w, router_b, w_gate_up, b_gate_up, w_down, b_down):
    n, d = h.shape
    logits = (h @ router_w + router_b).astype(jnp.float32)
    top_logit, top_e = lax.top_k(logits, TOP_K)
    probs = jax.nn.softmax(top_logit, axis=-1)
    a = n * TOP_K
    flat_e = top_e.reshape(a)
    flat_tok = jnp.arange(a, dtype=jnp.int32) // TOP_K
    flat_p = probs.reshape(a)
    order = jnp.argsort(flat_e)
    se = flat_e[order]
    counts = jnp.bincount(flat_e, length=N_EXPERTS)
    pcounts = (counts + EXPERT_BLOCK - 1) // EXPERT_BLOCK * EXPERT_BLOCK
    start = jnp.cumsum(counts) - counts
    pend = jnp.cumsum(pcounts)
    pstart = pend - pcounts
    dest = pstart[se] + jnp.arange(a, dtype=jnp.int32) - start[se]
    n_blocks = -(-a // EXPERT_BLOCK) + N_EXPERTS
    slots = n_blocks * EXPERT_BLOCK
    slot_tok = jnp.full((slots,), n, jnp.int32).at[dest].set(flat_tok[order])
    slot_p = jnp.zeros((slots,), jnp.float32).at[dest].set(flat_p[order])
    block_e = jnp.minimum(jnp.searchsorted(pend // EXPERT_BLOCK, jnp.arange(n_blocks), side='right'), N_EXPERTS - 1)
    h_pad = jnp.concatenate([h, jnp.zeros((1, d), h.dtype)], axis=0)
    xb = h_pad[slot_tok].reshape(n_blocks, EXPERT_BLOCK, d)

    def expert_block(args):
        xe, e = args
        gu = xe @ w_gate_up[e] + b_gate_up[e]
        gate, up = gu[:, :D_EXPERT], gu[:, D_EXPERT:]
        gate = jnp.minimum(gate, SWIGLU_LIMIT)
        up = jnp.clip(up, -SWIGLU_LIMIT, SWIGLU_LIMIT)
        glu = gate * jax.nn.sigmoid(gate * SWIGLU_ALPHA)
        return ((up + 1.0) * glu) @ w_down[e] + b_down[e]

    yb = lax.map(expert_block, (xb, block_e)).reshape(slots, d)
    out = jnp.zeros((n + 1, d), yb.dtype).at[slot_tok].add(yb * slot_p[:, None].astype(yb.dtype))
    return out[:n]


def setup_inputs(seed: int = 0) -> dict:
    key = jax.random.key(seed)
    it = iter(jax.random.split(key, 64))
    nrm = lambda shape, scale: jax.random.normal(next(it), shape, jnp.float32) * scale
    D, L = D_MODEL, DEPTH
    a0 = jax.random.uniform(next(it), (L, 2, RNN_W), jnp.float32, 0.9, 0.999)
    s0 = a0 ** (1.0 / RG_C)
    dt0 = jnp.exp(jax.random.uniform(next(it), (L, 2, SSM_HEADS), jnp.float32, math.log(1e-3), math.log(1e-1)))
    return {
        'x': nrm((BATCH, SEQ, D), 1.0),
        'c': nrm((BATCH, D), 1.0),
        'ctx': nrm((BATCH, CTX_LEN, D), 1.0),
        'c_ctx': nrm((D,), 1.0),
        'w_ada': nrm((L, D, 6 * D), 0.5 * D ** -0.5),
        'b_ada': nrm((L, 6 * D), 0.02),
        'w_in': nrm((L, D, IN_W), D ** -0.5),
        'rnn_conv_w': nrm((L, CONV_W, RNN_W), CONV_W ** -0.5),
        'rnn_conv_b': nrm((L, RNN_W), 0.02),
        'rnn_wa': nrm((L, 2, RNN_BLOCKS, RNN_BLOCK, RNN_BLOCK), RNN_BLOCK ** -0.5),
        'rnn_ba': nrm((L, 2, RNN_W), 0.02),
        'rnn_wx': nrm((L, 2, RNN_BLOCKS, RNN_BLOCK, RNN_BLOCK), RNN_BLOCK ** -0.5),
        'rnn_bx': nrm((L, 2, RNN_W), 0.02),
        'rnn_lam': jnp.log(s0) - jnp.log1p(-s0),
        'att_lambda': nrm((L, 4, ATT_DIM), 0.1),
        'att_subln': 1.0 + nrm((L, 2 * ATT_DIM), 0.02),
        'pool_w': nrm((L, POOL_GROUPS, POOL_GW, POOL_GW), POOL_GW ** -0.5),
        'pool_b': nrm((L, POOL_W), 0.02),
        'pool_scale': 1.0 + nrm((L, POOL_W), 0.02),
        'ssm_conv_w': nrm((L, CONV_W, SSM_XBC_W), CONV_W ** -0.5),
        'ssm_conv_b': nrm((L, SSM_XBC_W), 0.02),
        'ssm_dt_bias': dt0 + jnp.log(-jnp.expm1(-dt0)),
        'ssm_a_log': jnp.log(jax.random.uniform(next(it), (L, 2, SSM_HEADS), jnp.float32, 1.0, 16.0)),
        'ssm_d': 1.0 + nrm((L, 2, SSM_HEADS), 0.02),
        'ssm_norm': 1.0 + nrm((L, SSM_W), 0.02),
        'w_branch': nrm((L, N_BRANCH, BRANCH_W, D), BRANCH_W ** -0.5),
        'w_out': nrm((L, D, D), DN_BETA * D ** -0.5),
        'ln1_g': 1.0 + nrm((L, D), 0.02),
        'ln1_b': nrm((L, D), 0.02),
        'ln2_g': 1.0 + nrm((L, D), 0.02),
        'ln2_b': nrm((L, D), 0.02),
        'router_w': nrm((L, D, N_EXPERTS), D ** -0.5),
        'router_b': nrm((L, N_EXPERTS), 0.01),
        'w_gate_up': nrm((L, N_EXPERTS, D, 2 * D_EXPERT), D ** -0.5),
        'b_gate_up': nrm((L, N_EXPERTS, 2 * D_EXPERT), 0.02),
        'w_down': nrm((L, N_EXPERTS, D_EXPERT, D), DN_BETA * D_EXPERT ** -0.5),
        'b_down': nrm((L, N_EXPERTS, D), 0.02),
    }


def reference(x, c, ctx, c_ctx, w_ada, b_ada, w_in, rnn_conv_w, rnn_conv_b, rnn_wa, rnn_ba, rnn_wx, rnn_bx, rnn_lam, att_lambda, att_subln, pool_w, pool_b, pool_scale, ssm_conv_w, ssm_conv_b, ssm_dt_bias, ssm_a_log, ssm_d, ssm_norm, w_branch, w_out, ln1_g, ln1_b, ln2_g, ln2_b, router_w, router_b, w_gate_up, b_gate_up, w_down, b_down):
    n_lat = x.shape[1]
    cos, sin = axial_rope_tables(n_lat)
    xl, xc = x, ctx
    for li in range(DEPTH):
        need_ctx = li < DEPTH - 1
        mod_l = (jax.nn.silu(c) @ w_ada[li] + b_ada[li])[:, None, :]
        mod_c = (jax.nn.silu(c_ctx) @ w_ada[li] + b_ada[li])[None, None, :]
        sh1, sc1, g1, sh2, sc2, g2 = jnp.split(mod_l, 6, axis=-1)
        csh1, csc1, cg1, csh2, csc2, cg2 = jnp.split(mod_c, 6, axis=-1)

        hl = modulate(xl, sh1, sc1)
        hc = modulate(xc, csh1, csc1)
        (rx_l, rg_l, q_l, k_l, v_l, pu_l, z_l, xbc_l, dt_l, mg_l) = jnp.split(hl @ w_in[li], IN_SPLITS, axis=-1)
        (rx_c, rg_c, q_c, k_c, v_c, pu_c, z_c, xbc_c, dt_c, mg_c) = jnp.split(hc @ w_in[li], IN_SPLITS, axis=-1)
        rnn_l, rnn_c = rglru_branch(rx_c, rx_l, rg_c, rg_l, rnn_conv_w[li], rnn_conv_b[li], rnn_wa[li], rnn_ba[li], rnn_wx[li], rnn_bx[li], rnn_lam[li], need_ctx)
        att_l, att_c = diff_attention_branch(q_c, k_c, v_c, q_l, k_l, v_l, att_lambda[li], att_subln[li], li, cos, sin, need_ctx)
        pool_l = multiscale_pool(pu_l, pool_w[li], pool_b[li], pool_scale[li])
        ssm_l, ssm_c = ssd_branch(z_c, xbc_c, dt_c, z_l, xbc_l, dt_l, ssm_conv_w[li], ssm_conv_b[li], ssm_dt_bias[li], ssm_a_log[li], ssm_d[li], ssm_norm[li], need_ctx)
        y_l = merge_branches((rnn_l, att_l, pool_l, ssm_l), mg_l, w_branch[li], w_out[li])
        if need_ctx:
            pool_c = multiscale_pool(pu_c, pool_w[li], pool_b[li], pool_scale[li])
            y_c = merge_branches((rnn_c, att_c, pool_c, ssm_c), mg_c, w_branch[li], w_out[li])
            xc = layer_norm(DN_ALPHA * xc + cg1 * y_c, ln1_g[li], ln1_b[li])
        xl = layer_norm(DN_ALPHA * xl + g1 * y_l, ln1_g[li], ln1_b[li])

        hl2 = modulate(xl, sh2, sc2).reshape(-1, D_MODEL)
        if need_ctx:
            hc2 = modulate(xc, csh2, csc2).reshape(-1, D_MODEL)
            f = moe_ffn(jnp.concatenate([hc2, hl2], axis=0), router_w[li], router_b[li], w_gate_up[li], b_gate_up[li], w_down[li], b_down[li])
            f_c, f_l = f[:hc2.shape[0]], f[hc2.shape[0]:]
            xc = layer_norm(DN_ALPHA * xc + cg2 * f_c.reshape(xc.shape), ln2_g[li], ln2_b[li])
        else:
            f_l = moe_ffn(hl2, router_w[li], router_b[li], w_gate_up[li], b_gate_up[li], w_down[li], b_down[li])
        xl = layer_norm(DN_ALPHA * xl + g2 * f_l.reshape(xl.shape), ln2_g[li], ln2_b[li])
    return xl
```

```python
import os
import numpy as np
import concourse.bass as bass
import concourse.mybir as mybir

F32 = mybir.dt.float32
BF16 = mybir.dt.bfloat16
F32R = mybir.dt.float32r
I32 = mybir.dt.int32
AF = mybir.ActivationFunctionType
ALU = mybir.AluOpType
AX = mybir.AxisListType

NDS = 24


class Trk:
    __slots__ = ("w", "r")

    def __init__(self):
        self.w = {}
        self.r = {}


class KB:
    def __init__(self, nc, same_engine_raw=True):
        self.nc = nc
        self.eng = {"pe": nc.tensor, "act": nc.scalar, "dve": nc.vector,
                    "pool": nc.gpsimd, "sp": nc.sync}
        self.sems = {}
        self.cnt = {}
        for k in ("pe", "act", "dve", "pool"):
            self.sems[k] = nc.alloc_semaphore("s_" + k)
            self.cnt[k] = 0
        self.dq = {}
        for q in ("sp", "act", "pool"):
            self.dq[q] = {"sems": [], "vals": [0] * NDS, "nxt": 0}
        for q in ("sp", "pool"):
            for i in range(NDS):
                key = ("d", q, i)
                self.sems[key] = nc.alloc_semaphore("d_%s_%d" % (q, i))
                self.dq[q]["sems"].append(key)
        self.seen = {k: {} for k in self.eng}
        self.trk = {}
        self.manual = set()
        self.psum_names = set()
        self.same_engine_raw = same_engine_raw and os.environ.get('SE_SYNC', '1') != '0'
        self.same_engine_war = os.environ.get('SE_SYNC', '1') == '1'
        self.n_inst = 0
        self.n_wait = 0
        self._uid = 0

    def _reg(self, t):
        self.trk[t.name] = Trk()
        return t

    def sb(self, shape, dtype=F32, name=None):
        self._uid += 1
        name = "%s_%d" % (name or "t", self._uid)
        return self._reg(self.nc.alloc_sbuf_tensor(name, list(shape), dtype))

    def ps(self, shape, dtype=F32, name=None):
        self._uid += 1
        name = "%s_%d" % (name or "p", self._uid)
        t = self._reg(self.nc.alloc_psum_tensor(name, list(shape), dtype))
        self.psum_names.add(t.name)
        return t

    def dram(self, name, shape, dtype=F32, kind="Internal", manual=False):
        t = self.nc.dram_tensor(name, list(shape), dtype, kind=kind)
        if manual:
            self.manual.add(t.name)
        else:
            self.trk[t.name] = Trk()
        return t

    def _key(self, x):
        if isinstance(x, (str, tuple)):
            if x not in self.trk:
                self.trk[x] = Trk()
            return x
        t = getattr(x, "tensor", None)
        if t is None:
            return None
        n = t.name
        if n in self.manual:
            return None
        return n if n in self.trk else None

    def _wait(self, ek, semkey, val):
        s = self.seen[ek]
        if s.get(semkey, 0) >= val:
            return
        s[semkey] = val
        self.eng[ek].wait_ge(self.sems[semkey], val)
        self.n_wait += 1

    def _deps(self, ek, reads, writes, is_dma=False):
        for x in reads:
            kx = self._key(x)
            tr = self.trk.get(kx)
            if tr is None:
                continue
            for sk, (v, wek) in tr.w.items():
                if sk == ek and (ek == "pe" or not self.same_engine_raw):
                    continue
                self._wait(ek, sk, v)
            if kx in self.psum_names:
                for sk, (v, rek) in tr.r.items():
                    if sk != ek:
                        self._wait(ek, sk, v)
        for x in writes:
            tr = self.trk.get(self._key(x))
            if tr is None:
                continue
            for sk, (v, wek) in tr.w.items():
                if sk == ek and (ek == "pe" or not self.same_engine_war):
                    continue
                if is_dma and isinstance(sk, tuple):
                    continue
                self._wait(ek, sk, v)
            for sk, (v, rek) in tr.r.items():
                if sk == ek and (ek == "pe" or not self.same_engine_war):
                    continue
                self._wait(ek, sk, v)

    def _mark(self, ek, tok, reads, writes, is_dma=False):
        sk, v = tok
        for x in reads:
            tr = self.trk.get(self._key(x))
            if tr is not None:
                tr.r[sk] = (v, ek)
        for x in writes:
            tr = self.trk.get(self._key(x))
            if tr is not None:
                if is_dma:
                    tr.w = {k: e for k, e in tr.w.items() if isinstance(k, tuple)}
                    tr.w[sk] = (v, ek)
                else:
                    tr.w = {sk: (v, ek)}
                tr.r = {}

    @staticmethod
    def _split(kwargs):
        reads, writes = [], []
        for k, v in kwargs.items():
            if v is None or isinstance(v, (int, float, bool, str)):
                continue
            if not hasattr(v, "tensor"):
                continue
            if k in ("out", "accum_out", "ap"):
                writes.append(v)
            else:
                reads.append(v)
        return reads, writes

    def I(self, ek, meth, extra_reads=(), extra_writes=(), **kwargs):
        reads, writes = self._split(kwargs)
        reads += list(extra_reads)
        writes += list(extra_writes)
        self._deps(ek, reads, writes)
        ins = getattr(self.eng[ek], meth)(**kwargs)
        self.cnt[ek] += 1
        ins.then_inc(self.sems[ek], 1)
        self._mark(ek, (ek, self.cnt[ek]), reads, writes)
        self.n_inst += 1
        return ins

    def dma(self, out, in_, q="sp", extra_reads=(), extra_writes=(), **kw):
        d = self.dq[q]
        i = d["nxt"]
        d["nxt"] = (i + 1) % NDS
        key = d["sems"][i]
        if d["vals"][i] > 0:
            self._wait(q, key, d["vals"][i])
        reads, writes = [in_], [out]
        reads += list(extra_reads)
        writes += list(extra_writes)
        self._deps(q, reads, writes, is_dma=True)
        ins = self.eng[q].dma_start(out=out, in_=in_, **kw)
        d["vals"][i] += 16
        ins.then_inc(self.sems[key], 16)
        self._mark(q, (key, d["vals"][i]), reads, writes, is_dma=True)
        self.n_inst += 1
        return ins

    def finish(self, outs):
        for t in outs:
            tr = self.trk.get(self._key(t))
            if tr is not None:
                for sk, (v, ek) in tr.w.items():
                    self._wait("sp", sk, v)
        for q, d in self.dq.items():
            for i, key in enumerate(d["sems"]):
                if d["vals"][i] > 0:
                    self._wait("sp", key, d["vals"][i])

    def barrier(self):
        toks = [(ek, self.cnt[ek]) for ek in ("pe", "act", "dve", "pool") if self.cnt[ek] > 0]
        for q, d in self.dq.items():
            for i, key in enumerate(d["sems"]):
                if d["vals"][i] > 0:
                    toks.append((key, d["vals"][i]))
        for ek in ("pe", "act", "dve", "pool", "sp"):
            for sk, v in toks:
                if sk == ek:
                    continue
                self._wait(ek, sk, v)

    def sbs(self, stack, shape, dtype=F32, name=None):
        self._uid += 1
        name = "%s_%d" % (name or "s", self._uid)
        t = stack.enter_context(self.nc.sbuf_tensor(name, list(shape), dtype))
        self.trk[t.name] = Trk()
        return t


from contextlib import ExitStack
import math, os
from concourse.bass_utils import run_bass_kernel_spmd

D = 1024
T = 2304
NCTX = 256
NLAT = 2048
TT = [(0, 256), (256, 512), (768, 512), (1280, 512), (1792, 512)]
OFF = dict(rx=0, rg=512, q=1024, k=1536, v=2048, pu=2560, z=3072, xbc=3584, dt=4352, mg=4368)
ALPHA = 8.0 ** 0.25
NEXP = 32

WSPEC = [
    ("w_ada", [4, 1024, 6144]), ("b_ada", [4, 6144]), ("w_in", [4, 1024, 8464]),
    ("rnn_conv_w", [4, 4, 512]), ("rnn_conv_b", [4, 512]), ("rnn_wa", [4, 2, 8, 64, 64]),
    ("rnn_ba", [4, 2, 512]), ("rnn_wx", [4, 2, 8, 64, 64]), ("rnn_bx", [4, 2, 512]),
    ("rnn_lam", [4, 2, 512]), ("att_lambda", [4, 4, 64]), ("att_subln", [4, 128]),
    ("pool_w", [4, 4, 128, 128]), ("pool_b", [4, 512]), ("pool_scale", [4, 512]),
    ("ssm_conv_w", [4, 4, 768]), ("ssm_conv_b", [4, 768]), ("ssm_dt_bias", [4, 2, 8]),
    ("ssm_a_log", [4, 2, 8]), ("ssm_d", [4, 2, 8]), ("ssm_norm", [4, 512]),
    ("w_branch", [4, 4, 512, 1024]), ("w_out", [4, 1024, 1024]),
    ("ln1_g", [4, 1024]), ("ln1_b", [4, 1024]), ("ln2_g", [4, 1024]), ("ln2_b", [4, 1024]),
    ("router_w", [4, 1024, 32]), ("router_b", [4, 32]),
    ("w_gate_up", [4, 32, 1024, 2048]), ("b_gate_up", [4, 32, 2048]),
    ("w_down", [4, 32, 1024, 1024]), ("b_down", [4, 32, 1024]),
]
CSPEC = [("k_ident", [128, 128]), ("k_cs", [2, 128, 2048]), ("k_perm", [128, 128]),
         ("k_mask", [2, 128, 896]), ("k_esel", [16, 2048])]


def host_consts():
    ident = np.eye(128, dtype=np.float32)
    t = np.arange(NLAT)
    row = (t // 64).astype(np.float32)
    col = (t % 64).astype(np.float32)
    inv = (10000.0 ** (-np.arange(16, dtype=np.float32) / 16)).astype(np.float32)
    cs = np.zeros((2, 128, NLAT), np.float32)
    perm = np.zeros((128, 128), np.float32)
    for p in range(128):
        d = p % 64
        axis, half, fr = d // 32, (d % 32) // 16, d % 16
        pos = row if axis == 0 else col
        ang = (pos * inv[fr]).astype(np.float32)
        cs[0, p] = np.cos(ang)
        cs[1, p] = np.sin(ang) * (-1.0 if half == 0 else 1.0)
        partner = p + 16 if half == 0 else p - 16
        perm[partner, p] = 1.0
    cc = np.arange(-384, 512)[None, :]
    sp = np.arange(128)[:, None]
    mask = np.stack([(cc >= sp), (cc <= sp)]).astype(np.float32)
    esel = np.zeros((16, 16 * 128), np.float32)
    for j in range(16):
        esel[j, j * 128:(j + 1) * 128] = 1.0
    return dict(k_ident=ident, k_cs=cs, k_perm=perm, k_mask=mask, k_esel=esel)


def build(depth=4, dbg=(), nb=2, stop_after=None, stages=("rglru", "pool", "attn", "ssd", "merge"), wover=None, cut=99):
    nc = bass.Bass("TRN2", target_bir_lowering=False)
    k = KB(nc)
    I = k.I

    def ext(name, shape, kind="ExternalInput", dtype=F32):
        t = nc.dram_tensor(name, list(shape), dtype, kind=kind)
        k.trk[t.name] = Trk()
        return t

    x_in = ext("x", [2, NLAT, D])
    c_in = ext("c", [2, D])
    ctx_in = ext("ctx", [2, NCTX, D])
    cctx_in = ext("c_ctx", [D])
    W = {n: ext(n, (wover or {}).get(n, s)) for n, s in WSPEC}
    C = {n: ext(n, s) for n, s in CSPEC}
    out = ext("out", [2, NLAT, D], kind="ExternalOutput")
    xres = k.dram("xres", [2, T, D])
    modrow = k.dram("modrow", [4, 3, 6144])
    ysd = k.dram("ysd", [2, 4, 128, 4, T], BF16)
    DBG = {}
    for name, shape, dt_ in dbg:
        DBG[name] = ext("dbg_" + name, shape, kind="ExternalOutput", dtype=dt_)

    PB = [k.ps([128, 512], F32, "bank") for _ in range(8)]

    class Rot:
        def __init__(self, lst):
            self.l = lst
            self.i = 0

        def __call__(self):
            r = self.l[self.i % len(self.l)]
            self.i += 1
            return r

    ident = k.sb([128, 128], F32, "ident")
    k.dma(out=ident[:], in_=C["k_ident"].ap())
    ones = k.sb([128, 128], F32, "ones")
    I("dve", "memset", ap=ones[:], constant=1.0)
    onesb = k.sb([128, 128], BF16, "onesb")
    I("dve", "memset", ap=onesb[:], constant=1.0)
    cT = k.sb([128, 3, 8], F32, "cT")
    k.dma(out=cT[:, 0:2, :], in_=c_in.ap().rearrange("b (p j) -> p b j", j=8))
    k.dma(out=cT[:, 2, :], in_=cctx_in.ap().rearrange("(p j) -> p j", j=8))
    sT = k.sb([128, 3, 8], F32, "sT")
    I("act", "activation", out=sT[:], in_=cT[:], func=AF.Silu)
    for b in range(nb):
        k.dma(out=xres.ap()[b, 0:NCTX, :], in_=ctx_in.ap()[b])
        k.dma(out=xres.ap()[b, NCTX:T, :], in_=x_in.ap()[b])

    def dump(name, src_ap):
        if name in DBG:
            k.dma(out=DBG[name].ap(), in_=src_ap)

    def colvec(st, src1d, nch, name="cv"):
        t = k.sbs(st, [128, nch], F32, name)
        k.dma(out=t[:], in_=src1d.rearrange("(j p) -> p j", p=128), allow_slow_non_contiguous=True)
        return t

    def rstd_of(st_tile_var, eps, tmp):
        I("dve", "tensor_scalar", out=tmp, in0=st_tile_var, scalar1=float(eps), scalar2=None, op0=ALU.add)
        I("act", "activation", out=tmp, in_=tmp, func=AF.Sqrt)
        I("dve", "reciprocal", out=tmp, in_=tmp)

    def adaln(li):
        with ExitStack() as st:
            mrow = k.sbs(st, [3, 6144], F32, "mrow")
            brow = k.sbs(st, [3, 6144], F32, "brow")
            k.dma(out=brow[:], in_=W["b_ada"].ap()[li].partition_broadcast(3))
            wps = Rot([k.sbs(st, [128, 8, 512], F32, "wada") for _ in range(2)])
            wv = W["w_ada"].ap()[li].rearrange("(p j) n -> p j n", j=8)
            pr = Rot(PB[0:2])
            for n in range(12):
                wp = wps()
                k.dma(out=wp[:], in_=wv[:, :, n * 512:(n + 1) * 512])
                ps = pr()
                for j in range(8):
                    I("pe", "matmul", out=ps[0:3, :], lhsT=sT[:, :, j], rhs=wp[:, j, :], start=(j == 0), stop=(j == 7))
                I("dve", "tensor_tensor", out=mrow[:, n * 512:(n + 1) * 512], in0=ps[0:3, :], in1=brow[:, n * 512:(n + 1) * 512], op=ALU.add)
            k.dma(out=modrow.ap()[li], in_=mrow[:])
            k.barrier()

    def ln_mod_T(st, xt, dstT, col0, modc, modc1p, r, sc0, sh0, tmp_pool, extra_f32=None):
        stats, mv, rs, xn = tmp_pool
        for h in range(2):
            I("dve", "bn_stats", out=stats[:, h, :], in_=xt[:, h * 512:(h + 1) * 512])
        I("dve", "bn_aggr", out=mv[:], in_=stats[:])
        rstd_of(mv[:, 1:2], 1e-6, rs[:])
        I("dve", "tensor_scalar", out=xn[:], in0=xt[:], scalar1=mv[:, 0:1], scalar2=rs[:, 0:1], op0=ALU.subtract, op1=ALU.mult)
        for h in range(2):
            ps = PB[6 + h]
            for jj in range(4):
                j = h * 4 + jj
                I("pe", "transpose", out=ps[:, jj * 128:(jj + 1) * 128], in_=xn[:, j * 128:(j + 1) * 128], identity=ident[:])
            for jj in range(4):
                j = h * 4 + jj
                I("act", "activation", out=dstT[:, j, col0:col0 + 128], in_=ps[:, jj * 128:(jj + 1) * 128], func=AF.Identity,
                  scale=modc1p[:, sc0 + j, r:r + 1], bias=modc[:, sh0 + j, r:r + 1])
                if extra_f32 is not None:
                    I("act", "activation", out=extra_f32[:, j, :], in_=ps[:, jj * 128:(jj + 1) * 128], func=AF.Identity,
                      scale=modc1p[:, sc0 + j, r:r + 1], bias=modc[:, sh0 + j, r:r + 1])

    def load_modc(st, li):
        modc = k.sbs(st, [128, 48, 3], F32, "modc")
        modc1p = k.sbs(st, [128, 48, 3], F32, "modc1p")
        sM = ExitStack()
        mr = k.sbs(sM, [3, 6144], F32, "mr")
        k.dma(out=mr[:], in_=modrow.ap()[li])
        ps = PB[0]
        for j in range(48):
            I("pe", "transpose", out=ps[:, j * 3:(j + 1) * 3], in_=mr[0:3, j * 128:(j + 1) * 128], identity=ident[0:3, 0:3])
        I("dve", "tensor_copy", out=modc[:].rearrange("p a b -> p (a b)"), in_=ps[:, 0:144])
        I("dve", "tensor_scalar", out=modc1p[:].rearrange("p a b -> p (a b)"), in0=ps[:, 0:144], scalar1=1.0, scalar2=None, op0=ALU.add)
        k.barrier()
        sM.close()
        return modc, modc1p

    def mkstage(st, ncols, nbuf=2, kc=8):
        return Rot([k.sbs(st, [128, kc, ncols], F32, "wstg") for _ in range(nbuf)])

    def wload(dst, src, stage):
        s_ = stage()
        kc_, n_ = dst.shape[1], dst.shape[2]
        k.dma(out=s_[:, 0:kc_, 0:n_], in_=src)
        I("pool", "tensor_copy", out=dst, in_=s_[:, 0:kc_, 0:n_])

    def wview(name, li, c0, n):
        return W[name].ap()[li][:, c0:c0 + n].rearrange("(j p) n -> p j n", p=128)

    def proj(st, hT, li, c0, ncols, evac, wpool, tts=TT, banks=None):
        wt = wpool()
        wload(wt[:, :, 0:ncols], wview("w_in", li, c0, ncols), wpool.stage)
        banks = banks or Rot(PB[0:4])
        for (t0, n) in tts:
            ps = banks()
            for j in range(8):
                I("pe", "matmul", out=ps[0:ncols, 0:n], lhsT=wt[:, j, 0:ncols], rhs=hT[:, j, t0:t0 + n], start=(j == 0), stop=(j == 7))
            evac(ps, t0, n)

    SEG = [(0, NCTX), (NCTX, NLAT)]

    def conv_seg(xp, u, wc, bc):
        for si, (s0, L) in enumerate(SEG):
            p0 = 2 + s0 + 4 * si
            I("dve", "tensor_scalar", out=u[:, s0:s0 + L], in0=xp[:, p0 - 2:p0 - 2 + L], scalar1=wc[:, 0:1], scalar2=bc, op0=ALU.mult, op1=ALU.add)
            for kk in range(1, 4):
                I("dve", "scalar_tensor_tensor", out=u[:, s0:s0 + L], in0=xp[:, p0 - 2 + kk:p0 - 2 + kk + L], scalar=wc[:, kk:kk + 1],
                  in1=u[:, s0:s0 + L], op0=ALU.mult, op1=ALU.add)

    def pad_zero(xp):
        for (a, b_) in [(0, 2), (258, 262), (2310, 2312)]:
            I("pool", "memset", ap=xp[:, a:b_], constant=0.0)

    def xp_cols(t0, n):
        return (t0 + 2) if t0 < NCTX else (t0 + 6)

    def branch_rglru(st0, hT, li, b):
        with ExitStack() as st:
            wpool = Rot([k.sbs(st, [128, 8, 128], BF16, "wq") for _ in range(2)])
            wpool.stage = mkstage(st, 128)
            cw = k.sbs(st, [128, 4, 4], F32, "rcw")
            for kk in range(4):
                k.dma(out=cw[:, :, kk], in_=W["rnn_conv_w"].ap()[li, kk].rearrange("(j p) -> p j", p=128), allow_slow_non_contiguous=True)
            cb = colvec(st, W["rnn_conv_b"].ap()[li], 4, "rcb")
            ba = [colvec(st, W["rnn_ba"].ap()[li, d], 4, "rba") for d in range(2)]
            bx = [colvec(st, W["rnn_bx"].ap()[li, d], 4, "rbx") for d in range(2)]
            lam = [colvec(st, W["rnn_lam"].ap()[li, d], 4, "rlam") for d in range(2)]
            cpl = [k.sbs(st, [128, 4], F32, "cpl") for d in range(2)]
            cpl2 = [k.sbs(st, [128, 4], F32, "cpl2") for d in range(2)]
            tl = k.sbs(st, [128, 4], F32, "tl")
            for d in range(2):
                I("act", "activation", out=tl[:], in_=lam[d][:], func=AF.Abs)
                I("act", "activation", out=tl[:], in_=tl[:], func=AF.Exp, scale=-1.0)
                I("act", "activation", out=tl[:], in_=tl[:], func=AF.Ln, bias=1.0)
                I("dve", "tensor_scalar", out=cpl[d][:], in0=lam[d][:], scalar1=-1.0, scalar2=0.0, op0=ALU.mult, op1=ALU.max)
                I("dve", "tensor_tensor", out=cpl[d][:], in0=cpl[d][:], in1=tl[:], op=ALU.add)
                I("dve", "tensor_scalar", out=cpl2[d][:], in0=cpl[d][:], scalar1=-16.0, scalar2=None, op0=ALU.mult)
                I("dve", "tensor_scalar", out=cpl[d][:], in0=cpl[d][:], scalar1=-8.0, scalar2=None, op0=ALU.mult)
            wbd = [[k.sbs(st, [128, 128], F32, "wbd") for _ in range(2)] for _ in range(2)]
            xp = k.sbs(st, [128, T + 8], F32, "rxp")
            u = k.sbs(st, [128, T], F32, "ru")
            av = k.sbs(st, [128, T], F32, "ra")
            bv = k.sbs(st, [128, T], F32, "rb")
            hf = k.sbs(st, [128, T], F32, "rhf")
            it = Rot([k.sbs(st, [128, 512], F32, "rit") for _ in range(2)])
            yo = k.sbs(st, [128, T], BF16, "ryo")
            pad_zero(xp)
            for d in range(2):
                for w_ in range(2):
                    I("pool", "memset", ap=wbd[d][w_][:], constant=0.0)
            for fc in range(4):
                def ev_rx(ps, t0, n):
                    pc = xp_cols(t0, n)
                    I("act", "activation", out=xp[:, pc:pc + n], in_=ps[:, 0:n], func=AF.Copy)
                proj(st, hT, li, OFF["rx"] + fc * 128, 128, ev_rx, wpool)
                conv_seg(xp, u, cw[:, fc, :], cb[:, fc:fc + 1])
                for d in range(2):
                    for w_, nm in enumerate(("rnn_wa", "rnn_wx")):
                        for blk in range(2):
                            k.dma(out=wbd[d][w_][blk * 64:(blk + 1) * 64, blk * 64:(blk + 1) * 64], in_=W[nm].ap()[li, d, fc * 2 + blk])
                for d in range(2):
                    banks = Rot(PB[4:6])
                    for (t0, n) in TT:
                        psa = banks()
                        I("pe", "matmul", out=psa[:, 0:n], lhsT=wbd[d][0][:], rhs=u[:, t0:t0 + n], start=True, stop=True)
                        rt = it()
                        I("act", "activation", out=rt[:, 0:n], in_=psa[:, 0:n], func=AF.Sigmoid, bias=ba[d][:, fc:fc + 1])
                        I("act", "activation", out=av[:, t0:t0 + n], in_=rt[:, 0:n], func=AF.Exp, scale=cpl[d][:, fc:fc + 1])
                        I("act", "activation", out=bv[:, t0:t0 + n], in_=rt[:, 0:n], func=AF.Exp, scale=cpl2[d][:, fc:fc + 1])
                        psx = banks()
                        I("pe", "matmul", out=psx[:, 0:n], lhsT=wbd[d][1][:], rhs=u[:, t0:t0 + n], start=True, stop=True)
                        it2 = it()
                        I("act", "activation", out=it2[:, 0:n], in_=psx[:, 0:n], func=AF.Sigmoid, bias=bx[d][:, fc:fc + 1])
                        I("pool", "tensor_tensor", out=it2[:, 0:n], in0=it2[:, 0:n], in1=u[:, t0:t0 + n], op=ALU.mult)
                        I("dve", "tensor_scalar", out=bv[:, t0:t0 + n], in0=bv[:, t0:t0 + n], scalar1=-1.0, scalar2=1.0, op0=ALU.mult, op1=ALU.add)
                        I("act", "activation", out=bv[:, t0:t0 + n], in_=bv[:, t0:t0 + n], func=AF.Sqrt)
                        I("dve", "tensor_tensor", out=bv[:, t0:t0 + n], in0=bv[:, t0:t0 + n], in1=it2[:, 0:n], op=ALU.mult)
                    if d == 0:
                        I("dve", "tensor_tensor_scan", out=hf[:, :], data0=av[:, :], data1=bv[:, :], initial=0.0, op0=ALU.mult, op1=ALU.add)
                    else:
                        hb = xp
                        I("dve", "tensor_tensor_scan", out=hb[:, 255::-1], data0=av[:, 255::-1], data1=bv[:, 255::-1], initial=0.0, op0=ALU.mult, op1=ALU.add)
                        I("dve", "tensor_tensor_scan", out=hb[:, T - 1:255:-1], data0=av[:, T - 1:255:-1], data1=bv[:, T - 1:255:-1],
                          initial=hb[:, 0:1], op0=ALU.mult, op1=ALU.add)
                        I("dve", "tensor_tensor", out=hf[:, :], in0=hf[:, :], in1=hb[:, 0:T], op=ALU.add)
                def ev_rg(ps, t0, n):
                    g = it()
                    g2 = it()
                    I("act", "activation", out=g[:, 0:n], in_=ps[:, 0:n], func=AF.Square)
                    I("dve", "tensor_scalar", out=g[:, 0:n], in0=g[:, 0:n], scalar1=0.044715, scalar2=1.0, op0=ALU.mult, op1=ALU.add)
                    I("dve", "tensor_tensor", out=g[:, 0:n], in0=g[:, 0:n], in1=ps[:, 0:n], op=ALU.mult)
                    I("act", "activation", out=g[:, 0:n], in_=g[:, 0:n], func=AF.Sigmoid, scale=2.0 * math.sqrt(2.0 / math.pi))
                    I("dve", "tensor_tensor", out=g2[:, 0:n], in0=g[:, 0:n], in1=ps[:, 0:n], op=ALU.mult)
                    I("pool", "tensor_tensor", out=yo[:, t0:t0 + n], in0=g2[:, 0:n], in1=hf[:, t0:t0 + n], op=ALU.mult)
                proj(st, hT, li, OFF["rg"] + fc * 128, 128, ev_rg, wpool)
                k.dma(out=ysd.ap()[b, 0, :, fc, :], in_=yo[:])
                if fc == 0:
                    dump("rnn0", yo[:])
                pad_zero(xp)
            k.barrier()

    def branch_pool(st0, hT, li, b):
        with ExitStack() as st:
            wpool = Rot([k.sbs(st, [128, 8, 128], BF16, "wq") for _ in range(2)])
            wpool.stage = mkstage(st, 128)
            pb = colvec(st, W["pool_b"].ap()[li], 4, "pb")
            psc = colvec(st, W["pool_scale"].ap()[li], 4, "psc")
            I("dve", "tensor_tensor", out=pb[:], in0=pb[:], in1=psc[:], op=ALU.mult)
            pw = k.sbs(st, [128, 4, 128], BF16, "pw")
            wload(pw[:], W["pool_w"].ap()[li].rearrange("g c e -> c g e"), wpool.stage)
            PADL = 16
            segoff = [PADL, PADL + NCTX + 32]
            WP = NCTX + NLAT + 64 + 16
            xp = k.sbs(st, [128, WP], F32, "pxp")
            s1 = k.sbs(st, [128, WP], F32, "ps1")
            s2 = k.sbs(st, [128, WP], F32, "ps2")
            mix = k.sbs(st, [128, T], BF16, "pmix")
            yo = k.sbs(st, [128, T], BF16, "pyo")
            I("pool", "memset", ap=xp[:], constant=0.0)
            for g in range(4):
                win = (2, 4, 8, 16)[g]

                def ev(ps, t0, n):
                    si = 0 if t0 < NCTX else 1
                    pc = segoff[si] + (t0 - SEG[si][0])
                    I("act", "activation", out=xp[:, pc:pc + n], in_=ps[:, 0:n], func=AF.Copy)
                proj(st, hT, li, OFF["pu"] + g * 128, 128, ev, wpool)
                for si, (s0, L) in enumerate(SEG):
                    o = segoff[si]
                    lo, hi = o - 12, o + L + 12
                    I("dve", "tensor_tensor", out=s1[:, lo:hi], in0=xp[:, lo:hi], in1=xp[:, lo - 1:hi - 1], op=ALU.add)
                    cur, oth = s1, s2
                    sh = 1
                    while sh * 2 < win:
                        lo, hi = lo + sh, hi - sh
                        I("dve", "tensor_tensor", out=oth[:, lo:hi], in0=cur[:, lo + sh:hi + sh], in1=cur[:, lo - sh:hi - sh], op=ALU.add)
                        cur, oth = oth, cur
                        sh *= 2
                    res = oth
                    I("dve", "tensor_scalar", out=res[:, o:o + L], in0=cur[:, o:o + L], scalar1=1.0 / win, scalar2=None, op0=ALU.mult)
                    for tpos in range(L):
                        lo_ = max(tpos - win // 2, 0)
                        hi_ = min(tpos + win - 1 - win // 2, L - 1)
                        cnt = hi_ - lo_ + 1
                        if cnt != win:
                            I("dve", "tensor_scalar", out=res[:, o + tpos:o + tpos + 1], in0=cur[:, o + tpos:o + tpos + 1], scalar1=1.0 / cnt, scalar2=None, op0=ALU.mult)
                        if tpos == win and L > 4 * win:
                            pass
                    I("dve", "tensor_tensor", out=mix[:, s0:s0 + L], in0=res[:, o:o + L], in1=xp[:, o:o + L], op=ALU.subtract)
                banks = Rot(PB[4:6])
                for (t0, n) in TT:
                    ps = banks()
                    I("pe", "matmul", out=ps[:, 0:n], lhsT=pw[:, g, :], rhs=mix[:, t0:t0 + n], start=True, stop=True)
                    I("act", "activation", out=yo[:, t0:t0 + n], in_=ps[:, 0:n], func=AF.Identity, scale=psc[:, g:g + 1], bias=pb[:, g:g + 1])
                k.dma(out=ysd.ap()[b, 2, :, g, :], in_=yo[:])
                if g == 3:
                    dump("pool3", yo[:])
            k.barrier()

    def branch_attn(st0, hT, li, b):
        lam_init = 0.8 - 0.6 * math.exp(-0.3 * li)
        with ExitStack() as st:
            wpool = Rot([k.sbs(st, [128, 8, 512], BF16, "wq") for _ in range(2)])
            wpool.stage = mkstage(st, 512, nbuf=1)
            cosT = k.sbs(st, [128, NLAT], F32, "cos")
            sinT = k.sbs(st, [128, NLAT], F32, "sin")
            k.dma(out=cosT[:], in_=C["k_cs"].ap()[0])
            k.dma(out=sinT[:], in_=C["k_cs"].ap()[1])
            perm = k.sbs(st, [128, 128], BF16, "perm")
            wload(perm[:].rearrange("p (a n) -> p a n", a=1), C["k_perm"].ap().rearrange("p (a n) -> p a n", a=1), wpool.stage)
            lv = k.sbs(st, [128, 256], F32, "lv")
            k.dma(out=lv[:], in_=W["att_lambda"].ap()[li].rearrange("a b -> (a b)").partition_broadcast(128))
            l2 = k.sbs(st, [128, 2], F32, "l2")
            lt = k.sbs(st, [128, 64], F32, "lt")
            for i2 in range(2):
                I("dve", "tensor_tensor", out=lt[:], in0=lv[:, i2 * 128:i2 * 128 + 64], in1=lv[:, i2 * 128 + 64:i2 * 128 + 128], op=ALU.mult)
                I("dve", "reduce_sum", out=l2[:, i2:i2 + 1], in_=lt[:], axis=AX.X)
            I("act", "activation", out=l2[:], in_=l2[:], func=AF.Exp)
            nlam = k.sbs(st, [128, 1], F32, "nlam")
            I("dve", "tensor_tensor", out=nlam[:], in0=l2[:, 1:2], in1=l2[:, 0:1], op=ALU.subtract)
            I("dve", "tensor_scalar", out=nlam[:], in0=nlam[:], scalar1=-lam_init, scalar2=None, op0=ALU.add)
            sw = k.sbs(st, [128, 1], F32, "sw")
            k.dma(out=sw[:], in_=W["att_subln"].ap()[li].rearrange("(p o) -> p o", o=1))
            I("dve", "tensor_scalar", out=sw[:], in0=sw[:], scalar1=1.0 - lam_init, scalar2=None, op0=ALU.mult)
            vt = k.sbs(st, [128, 18, 512], BF16, "vt")
            wv_ = wpool()
            wload(wv_[:], wview("w_in", li, OFF["v"], 512), wpool.stage)
            vb = Rot(PB[0:2])
            for tk in range(18):
                ps = vb()
                for j in range(8):
                    I("pe", "matmul", out=ps[:], lhsT=hT[:, j, tk * 128:(tk + 1) * 128], rhs=wv_[:, j, :], start=(j == 0), stop=(j == 7))
                I("act", "activation", out=vt[:, tk, :], in_=ps[:], func=AF.Copy)
            if cut <= 1:
                k.barrier()
                return
            qT = k.sbs(st, [128, T], BF16, "qT")
            kT = k.sbs(st, [128, T], BF16, "kT")
            tmpa = Rot([k.sbs(st, [128, 512], F32, "ta") for _ in range(3)])
            pT = Rot([k.sbs(st, [128, 512], BF16, "pT") for _ in range(4)])
            osb = k.sbs(st, [128, 512], F32, "osb")
            osq = k.sbs(st, [128, 512], F32, "osq")
            yo = k.sbs(st, [128, T], BF16, "ayo")
            for hd in range(4):
                wqk = wpool()
                wload(wqk[:, :, 0:128], wview("w_in", li, OFF["q"] + hd * 128, 128), wpool.stage)
                wload(wqk[:, :, 128:256], wview("w_in", li, OFF["k"] + hd * 128, 128), wpool.stage)
                banks = Rot(PB[0:2])
                for which, dst in ((0, qT), (1, kT)):
                    for (t0, n) in TT:
                        ps = banks()
                        for j in range(8):
                            I("pe", "matmul", out=ps[:, 0:n], lhsT=wqk[:, j, which * 128:(which + 1) * 128], rhs=hT[:, j, t0:t0 + n], start=(j == 0), stop=(j == 7))
                        if t0 < NCTX or os.environ.get("ROPE_OFF") == "1":
                            I("act", "activation", out=dst[:, t0:t0 + n], in_=ps[:, 0:n], func=AF.Copy)
                        else:
                            RS = int(os.environ.get("ROPE_STEP", "9"))
                            xb = pT()
                            I("act", "activation", out=xb[:, 0:n], in_=ps[:, 0:n], func=AF.Copy)
                            if RS <= 1:
                                I("act", "activation", out=dst[:, t0:t0 + n], in_=ps[:, 0:n], func=AF.Copy)
                                continue
                            ps2 = banks()
                            I("pe", "matmul", out=ps2[:, 0:n], lhsT=perm[:], rhs=xb[:, 0:n], start=True, stop=True)
                            if RS <= 2:
                                I("act", "activation", out=dst[:, t0:t0 + n], in_=ps2[:, 0:n], func=AF.Copy)
                                continue
                            ta = tmpa()
                            V = os.environ.get("RV", "0")
                            if V == "1":
                                I("dve", "tensor_tensor", out=ta[:, 0:n], in0=xb[:, 0:n], in1=cosT[:, t0 - NCTX:t0 - NCTX + n], op=ALU.mult)
                            elif V == "2":
                                I("dve", "tensor_copy", out=ta[:, 0:n], in_=cosT[:, t0 - NCTX:t0 - NCTX + n])
                            elif V == "3":
                                I("dve", "tensor_copy", out=ta[:, 0:n], in_=ps[:, 0:n])
                            else:
                                I("dve", "tensor_tensor", out=ta[:, 0:n], in0=ps[:, 0:n], in1=cosT[:, t0 - NCTX:t0 - NCTX + n], op=ALU.mult)
                            if RS <= 3:
                                I("act", "activation", out=dst[:, t0:t0 + n], in_=ta[:, 0:n], func=AF.Copy)
                                continue
                            tb = tmpa()
                            I("dve", "tensor_tensor", out=tb[:, 0:n], in0=ps2[:, 0:n], in1=sinT[:, t0 - NCTX:t0 - NCTX + n], op=ALU.mult)
                            I(os.environ.get("ROPE_ENG", "pool"), "tensor_tensor", out=dst[:, t0:t0 + n], in0=ta[:, 0:n], in1=tb[:, 0:n], op=ALU.add)
                if cut <= 2:
                    continue
                for (t0, n) in TT:
                    if cut <= 3 and t0 > 0:
                        continue
                    kchunks = [0, 1] if t0 < NCTX else list(range(18))
                    Ob = [PB[2], PB[3]]
                    Zb = [PB[4], PB[5]]
                    sb_ = Rot([PB[6], PB[7], PB[0], PB[1]])
                    steps = [(ci, kc, cm) for ci, kc in enumerate(kchunks) for cm in range(2)]

                    def s_mm(step):
                        ci, kc, cm = step
                        ps = sb_()
                        I("pe", "matmul", out=ps[:, 0:n], lhsT=kT[cm * 64:(cm + 1) * 64, kc * 128:(kc + 1) * 128],
                          rhs=qT[cm * 64:(cm + 1) * 64, t0:t0 + n], start=True, stop=True)
                        return ps
                    LA = 2
                    psq = [s_mm(steps[i_]) for i_ in range(min(LA, len(steps)))]
                    for si_, (ci, kc, cm) in enumerate(steps):
                        ps = psq.pop(0)
                        pt = pT()
                        I("act", "activation", out=pt[:, 0:n], in_=ps[:, 0:n], func=AF.Exp, scale=0.125)
                        if si_ + LA < len(steps):
                            psq.append(s_mm(steps[si_ + LA]))
                        I("pe", "matmul", out=Ob[cm][:, 0:n], lhsT=vt[:, kc, hd * 128:(hd + 1) * 128], rhs=pt[:, 0:n],
                          start=(ci == 0), stop=(ci == len(kchunks) - 1))
                        I("pe", "matmul", out=Zb[cm][:, 0:n], lhsT=onesb[:], rhs=pt[:, 0:n],
                          start=(ci == 0), stop=(ci == len(kchunks) - 1))
                    r0 = tmpa()
                    I("dve", "reciprocal", out=r0[:, 0:n], in_=Zb[0][:, 0:n])
                    I("dve", "tensor_tensor", out=osb[:, 0:n], in0=Ob[0][:, 0:n], in1=r0[:, 0:n], op=ALU.mult)
                    r1 = tmpa()
                    I("dve", "reciprocal", out=r1[:, 0:n], in_=Zb[1][:, 0:n])
                    I("dve", "scalar_tensor_tensor", out=r1[:, 0:n], in0=Ob[1][:, 0:n], scalar=nlam[:, 0:1], in1=r1[:, 0:n], op0=ALU.mult, op1=ALU.mult)
                    I("dve", "tensor_tensor", out=osb[:, 0:n], in0=osb[:, 0:n], in1=r1[:, 0:n], op=ALU.add)
                    I("act", "activation", out=osq[:, 0:n], in_=osb[:, 0:n], func=AF.Square)
                    ps = sb_()
                    I("pe", "matmul", out=ps[:, 0:n], lhsT=ones[:], rhs=osq[:, 0:n], start=True, stop=True)
                    rr = tmpa()
                    I("dve", "tensor_scalar", out=rr[:, 0:n], in0=ps[:, 0:n], scalar1=1.0 / 128.0, scalar2=1e-5, op0=ALU.mult, op1=ALU.add)
                    I("act", "activation", out=rr[:, 0:n], in_=rr[:, 0:n], func=AF.Sqrt)
                    I("dve", "reciprocal", out=rr[:, 0:n], in_=rr[:, 0:n])
                    I("dve", "scalar_tensor_tensor", out=yo[:, t0:t0 + n], in0=osb[:, 0:n], scalar=sw[:, 0:1], in1=rr[:, 0:n], op0=ALU.mult, op1=ALU.mult)
                k.dma(out=ysd.ap()[b, 1, :, hd, :], in_=yo[:])
                if hd == 0:
                    dump("att0", yo[:])
            k.barrier()

    def branch_ssd(st0, hT, li, b):
        with ExitStack() as st:
            wpool = Rot([k.sbs(st, [128, 8, 128], BF16, "wq") for _ in range(2)])
            wpool.stage = mkstage(st, 128)
            cw = k.sbs(st, [128, 6, 4], F32, "scw")
            for kk in range(4):
                k.dma(out=cw[:, :, kk], in_=W["ssm_conv_w"].ap()[li, kk].rearrange("(j p) -> p j", p=128), allow_slow_non_contiguous=True)
            cb = colvec(st, W["ssm_conv_b"].ap()[li], 6, "scb")
            nw = colvec(st, W["ssm_norm"].ap()[li], 4, "snw")
            dtb = k.sbs(st, [16, 1], F32, "dtb")
            k.dma(out=dtb[:], in_=W["ssm_dt_bias"].ap()[li].rearrange("d (h o) -> (d h) o", o=1))
            na = k.sbs(st, [16, 1], F32, "na")
            k.dma(out=na[:], in_=W["ssm_a_log"].ap()[li].rearrange("d (h o) -> (d h) o", o=1))
            I("act", "activation", out=na[:], in_=na[:], func=AF.Exp)
            I("dve", "tensor_scalar", out=na[:], in0=na[:], scalar1=-1.0, scalar2=None, op0=ALU.mult)
            dsk2 = k.sbs(st, [128, 2, 4], F32, "dsk2")
            for d in range(2):
                for hh in range(8):
                    k.dma(out=dsk2[(hh % 2) * 64:(hh % 2) * 64 + 64, d, hh // 2:hh // 2 + 1],
                          in_=W["ssm_d"].ap()[li, d, hh:hh + 1].partition_broadcast(64))
            dsk = k.sbs(st, [128, 4], F32, "dsk")
            I("dve", "tensor_tensor", out=dsk[:], in0=dsk2[:, 0, :], in1=dsk2[:, 1, :], op=ALU.add)
            esel = k.sbs(st, [16, 2048], F32, "esel")
            k.dma(out=esel[:], in_=C["k_esel"].ap())
            mask = k.sbs(st, [128, 2, 896], F32, "mask")
            k.dma(out=mask[:], in_=C["k_mask"].ap().rearrange("a p c -> p a c"))
            cT_ = k.sbs(st, [16, T], F32, "cTs")
            negc = k.sbs(st, [128, 18, 16], F32, "negc")
            dtk = k.sbs(st, [128, 18, 16], F32, "dtk")
            xsT = k.sbs(st, [128, 4, T], BF16, "xsT")
            xtok = k.sbs(st, [128, 18, 512], BF16, "xtok")
            BT = k.sbs(st, [128, T], BF16, "BT")
            CT = k.sbs(st, [128, T], BF16, "CT")
            k_zsT = k.sbs(st, [128, 4, T], BF16, "zsT")
            sA = ExitStack()
            dtT = k.sbs(sA, [16, T], F32, "dtT")
            tmp16 = k.sbs(sA, [16, T], F32, "tmp16")

            def ev_dt(ps, t0, n):
                I("act", "activation", out=dtT[:, t0:t0 + n], in_=ps[0:16, 0:n], func=AF.Identity, bias=dtb[:, 0:1])
            proj(st, hT, li, OFF["dt"], 16, ev_dt, wpool)
            I("act", "activation", out=tmp16[:], in_=dtT[:], func=AF.Abs)
            I("act", "activation", out=tmp16[:], in_=tmp16[:], func=AF.Exp, scale=-1.0)
            I("act", "activation", out=tmp16[:], in_=tmp16[:], func=AF.Ln, bias=1.0)
            I("dve", "scalar_tensor_tensor", out=dtT[:], in0=dtT[:], scalar=0.0, in1=tmp16[:], op0=ALU.max, op1=ALU.add)
            I("dve", "tensor_scalar", out=tmp16[:], in0=dtT[:], scalar1=na[:, 0:1], scalar2=None, op0=ALU.mult)
            zer = k.sbs(sA, [16, T], F32, "zer")
            I("pool", "memset", ap=zer[:], constant=1.0)
            I("dve", "tensor_tensor_scan", out=cT_[:, 0:NCTX], data0=zer[:, 0:NCTX], data1=tmp16[:, 0:NCTX], initial=0.0, op0=ALU.mult, op1=ALU.add)
            I("dve", "tensor_tensor_scan", out=cT_[:, NCTX:T], data0=zer[:, NCTX:T], data1=tmp16[:, NCTX:T], initial=cT_[:, NCTX - 1:NCTX], op0=ALU.mult, op1=ALU.add)
            crT = k.sbs(sA, [16, T], F32, "crT")
            I("dve", "tensor_tensor", out=crT[:], in0=tmp16[:], in1=cT_[:], op=ALU.subtract)
            I("dve", "tensor_scalar", out=crT[:, 0:NCTX], in0=crT[:, 0:NCTX], scalar1=cT_[:, NCTX - 1:NCTX], scalar2=None, op0=ALU.add)
            I("dve", "tensor_scalar", out=crT[:, NCTX:T], in0=crT[:, NCTX:T], scalar1=cT_[:, T - 1:T], scalar2=cT_[:, NCTX - 1:NCTX], op0=ALU.add, op1=ALU.add)
            rsel = k.sbs(sA, [16, 1], F32, "rsel")
            I("dve", "tensor_copy", out=rsel[:], in_=esel[:, 8 * 128:8 * 128 + 1])
            for j in range(9, 16):
                I("dve", "tensor_tensor", out=rsel[:], in0=rsel[:], in1=esel[:, j * 128:j * 128 + 1], op=ALU.add)
            I("dve", "tensor_tensor", out=crT[:], in0=crT[:], in1=cT_[:], op=ALU.subtract)
            I("dve", "scalar_tensor_tensor", out=cT_[:], in0=crT[:], scalar=rsel[:, 0:1], in1=cT_[:], op0=ALU.mult, op1=ALU.add)
            for tk in range(18):
                ps = PB[0]
                I("pe", "transpose", out=ps[:, 0:16], in_=cT_[0:16, tk * 128:(tk + 1) * 128], identity=ident[0:16, 0:16])
                I("pe", "transpose", out=ps[:, 16:32], in_=dtT[0:16, tk * 128:(tk + 1) * 128], identity=ident[0:16, 0:16])
                I("dve", "tensor_scalar", out=negc[:, tk, :], in0=ps[:, 0:16], scalar1=-1.0, scalar2=None, op0=ALU.mult)
                I("act", "activation", out=dtk[:, tk, :], in_=ps[:, 16:32], func=AF.Copy)
            k.barrier()
            sA.close()
            sB = ExitStack()
            xp = k.sbs(sB, [128, T + 8], F32, "sxp")
            u = k.sbs(sB, [128, T], F32, "su")
            pad_zero(xp)
            for fc in range(6):
                def ev_x(ps, t0, n):
                    pc = xp_cols(t0, n)
                    I("act", "activation", out=xp[:, pc:pc + n], in_=ps[:, 0:n], func=AF.Copy)
                proj(st, hT, li, OFF["xbc"] + fc * 128, 128, ev_x, wpool)
                conv_seg(xp, u, cw[:, fc, :], cb[:, fc:fc + 1])
                I("act", "activation", out=u[:], in_=u[:], func=AF.Silu)
                if fc < 4:
                    I("pool", "tensor_copy", out=xsT[:, fc, :], in_=u[:])
                    for tk in range(18):
                        ps = PB[1 + tk % 2]
                        I("pe", "transpose", out=ps[:, 0:128], in_=u[:, tk * 128:(tk + 1) * 128], identity=ident[:])
                        I("act", "activation", out=xtok[:, tk, fc * 128:(fc + 1) * 128], in_=ps[:, 0:128], func=AF.Copy)
                elif fc == 4:
                    I("pool", "tensor_copy", out=BT[:], in_=u[:])
                else:
                    I("pool", "tensor_copy", out=CT[:], in_=u[:])
            zsT = k_zsT
            for ch in range(4):
                def ev_z(ps, t0_, n_, ch=ch):
                    I("act", "activation", out=zsT[:, ch, t0_:t0_ + n_], in_=ps[:, 0:n_], func=AF.Silu)
                proj(st, hT, li, OFF["z"] + ch * 128, 128, ev_z, wpool)
            k.barrier()
            sB.close()
            maskneg = k.sbs(st, [128, 2, 896], F32, "maskneg")
            I("pool", "tensor_scalar", out=maskneg[:], in0=mask[:], scalar1=-1.0, scalar2=30000.0, op0=ALU.add, op1=ALU.mult)
            rowbc = [k.sbs(st, [128, 512], F32, "rowbc") for _ in range(8)]
            Lt = Rot([k.sbs(st, [128, 512], F32, "Lt") for _ in range(3)])
            Mt = Rot([k.sbs(st, [128, 512], BF16, "Mt") for _ in range(3)])
            gsb = [k.sbs(st, [128, 512], F32, "gsb") for _ in range(2)]
            gsq = k.sbs(st, [128, 512], F32, "gsq")
            zs = Rot([k.sbs(st, [128, 512], F32, "zs") for _ in range(2)])
            yo = [k.sbs(st, [128, T], BF16, "syo") for _ in range(4)]
            for (t0, n) in TT:
                lt_is_ctx = t0 < NCTX
                for g in range(2):
                    for d in range(2):
                        for hh in range(4):
                            j = d * 8 + g * 4 + hh
                            ps = PB[(d * 4 + hh) % 2]
                            I("pe", "matmul", out=ps[:, 0:n], lhsT=esel[:, j * 128:(j + 1) * 128], rhs=cT_[:, t0:t0 + n], start=True, stop=True)
                            I("act", "activation", out=rowbc[d * 4 + hh][:, 0:n], in_=ps[:, 0:n], func=AF.Copy)
                    ybank = [PB[2], PB[3]]
                    contrib = []
                    for sc in range(18):
                        s_is_ctx = sc < 2
                        for d in range(2):
                            if lt_is_ctx:
                                if not s_is_ctx:
                                    continue
                                contrib.append((sc, d, "diag", sc))
                            else:
                                if s_is_ctx:
                                    contrib.append((sc, d, "full", 0))
                                    continue
                                i_ = sc - 2
                                jt = (t0 - NCTX) // 512
                                if 4 * jt <= i_ <= 4 * jt + 3:
                                    contrib.append((sc, d, "diag", i_ - 4 * jt))
                                elif (d == 0 and i_ < 4 * jt) or (d == 1 and i_ > 4 * jt + 3):
                                    contrib.append((sc, d, "full", 0))
                    first = {hh: True for hh in range(4)}
                    last_idx = len(contrib) - 1
                    cbb = Rot(PB[4:6])
                    cur_sc = None
                    cbps = None
                    for ci, (sc, d, kind, rel) in enumerate(contrib):
                        if sc != cur_sc:
                            cur_sc = sc
                            cbps = cbb()
                            I("pe", "matmul", out=cbps[:, 0:n], lhsT=BT[g * 64:(g + 1) * 64, sc * 128:(sc + 1) * 128],
                              rhs=CT[g * 64:(g + 1) * 64, t0:t0 + n], start=True, stop=True)
                        for hh in range(4):
                            hidx = g * 4 + hh
                            j = d * 8 + hidx
                            rb = rowbc[d * 4 + hh]
                            L_ = Lt()
                            if kind == "full":
                                I("act", "activation", out=L_[:, 0:n], in_=rb[:, 0:n], func=AF.Exp, bias=negc[:, sc, j:j + 1])
                            else:
                                mo = 384 - rel * 128
                                I("dve", "scalar_tensor_tensor", out=L_[:, 0:n], in0=rb[:, 0:n], scalar=negc[:, sc, j:j + 1], in1=maskneg[:, d, mo:mo + n], op0=ALU.add, op1=ALU.add)
                                I("act", "activation", out=L_[:, 0:n], in_=L_[:, 0:n], func=AF.Exp)
                            M_ = Mt()
                            I("dve", "scalar_tensor_tensor", out=M_[:, 0:n], in0=L_[:, 0:n], scalar=dtk[:, sc, j:j + 1], in1=cbps[:, 0:n], op0=ALU.mult, op1=ALU.mult)
                            is_last = (ci == last_idx)
                            yb = ybank[hh // 2]
                            I("pe", "matmul", out=yb[(hh % 2) * 64:(hh % 2) * 64 + 64, 0:n], lhsT=xtok[:, sc, hidx * 64:(hidx + 1) * 64],
                              rhs=M_[:, 0:n], start=first[hh], stop=is_last)
                            first[hh] = False
                    for cc in range(2):
                        ch = g * 2 + cc
                        gs = gsb[cc]
                        I("dve", "scalar_tensor_tensor", out=gs[:, 0:n], in0=xsT[:, ch, t0:t0 + n], scalar=dsk[:, ch:ch + 1], in1=ybank[cc][:, 0:n], op0=ALU.mult, op1=ALU.add)
                    for cc in range(2):
                        ch = g * 2 + cc
                        I("dve", "tensor_tensor", out=gsb[cc][:, 0:n], in0=gsb[cc][:, 0:n], in1=zsT[:, ch, t0:t0 + n], op=ALU.mult)
                    ps = PB[6]
                    for cc in range(2):
                        I("act", "activation", out=gsq[:, 0:n], in_=gsb[cc][:, 0:n], func=AF.Square)
                        I("pe", "matmul", out=ps[:, 0:n], lhsT=ones[:], rhs=gsq[:, 0:n], start=(cc == 0), stop=(cc == 1))
                    rr = zs()
                    I("dve", "tensor_scalar", out=rr[:, 0:n], in0=ps[:, 0:n], scalar1=1.0 / 256.0, scalar2=1e-5, op0=ALU.mult, op1=ALU.add)
                    I("act", "activation", out=rr[:, 0:n], in_=rr[:, 0:n], func=AF.Sqrt)
                    I("dve", "reciprocal", out=rr[:, 0:n], in_=rr[:, 0:n])
                    for cc in range(2):
                        ch = g * 2 + cc
                        I("dve", "scalar_tensor_tensor", out=yo[ch][:, t0:t0 + n], in0=gsb[cc][:, 0:n], scalar=nw[:, ch:ch + 1], in1=rr[:, 0:n], op0=ALU.mult, op1=ALU.mult)
            for ch in range(4):
                k.dma(out=ysd.ap()[b, 3, :, ch, :], in_=yo[ch][:])
            dump("ssm0", yo[0][:])
            k.barrier()

    def bcast_row(st, src1d, n, name="bc"):
        t = k.sbs(st, [128, n], F32, name)
        k.dma(out=t[:], in_=src1d.partition_broadcast(128))
        return t

    def layer_norm_rows(xt, tmp_pool, gbc, bbc, eps):
        stats, mv, rs, xn = tmp_pool
        for h in range(2):
            I("dve", "bn_stats", out=stats[:, h, :], in_=xt[:, h * 512:(h + 1) * 512])
        I("dve", "bn_aggr", out=mv[:], in_=stats[:])
        rstd_of(mv[:, 1:2], eps, rs[:])
        I("dve", "tensor_scalar", out=xt[:], in0=xt[:], scalar1=mv[:, 0:1], scalar2=rs[:, 0:1], op0=ALU.subtract, op1=ALU.mult)
        I("pool", "tensor_tensor", out=xt[:], in0=xt[:], in1=gbc[:], op=ALU.mult)
        I("pool", "tensor_tensor", out=xt[:], in0=xt[:], in1=bbc[:], op=ALU.add)

    def mixer(li, b):
        with ExitStack() as st:
            modc, modc1p = load_modc(st, li)
            hT = k.sbs(st, [128, 8, T], BF16, "hT")
            with ExitStack() as s2:
                tmp_pool = (k.sbs(s2, [128, 2, 6], F32, "stats"), k.sbs(s2, [128, 2], F32, "mv"), k.sbs(s2, [128, 1], F32, "rs"), k.sbs(s2, [128, D], F32, "xn"))
                xts = Rot([k.sbs(s2, [128, D], F32, "xt") for _ in range(2)])
                for tk in range(18):
                    xt = xts()
                    k.dma(out=xt[:], in_=xres.ap()[b, tk * 128:(tk + 1) * 128, :])
                    r = 2 if tk < 2 else b
                    ln_mod_T(s2, xt, hT, tk * 128, modc, modc1p, r, 8, 0, tmp_pool)
                k.barrier()
            if b == 0 and li == 0:
                dump("hT", hT[:, 0, :])
            if "rglru" in stages:
                branch_rglru(st, hT, li, b)
            if "pool" in stages:
                branch_pool(st, hT, li, b)
            if "attn" in stages:
                branch_attn(st, hT, li, b)
            if "ssd" in stages:
                branch_ssd(st, hT, li, b)
            if "merge" not in stages:
                return
            with ExitStack() as s2:
              mT = k.sbs(s2, [128, 8, T], BF16, "mT")
              with ExitStack() as s3:
                ysT = [k.sbs(s3, [128, 4, T], BF16, "ysT") for _ in range(4)]
                for i in range(4):
                    k.dma(out=ysT[i][:], in_=ysd.ap()[b, i])
                wbr = Rot([k.sbs(s3, [128, 4, 4, 128], BF16, "wbr") for _ in range(2)])
                wmg = Rot([k.sbs(s3, [128, 8, 4, 128], BF16, "wmg") for _ in range(2)])
                gs = Rot([k.sbs(s3, [128, 512], F32, "gs") for _ in range(3)])
                mstage = mkstage(s3, 128, nbuf=2)
                acc = Rot([k.sbs(s3, [128, 512], F32, "macc") for _ in range(2)])
                pbk = Rot(PB[0:4])
                gbk = Rot(PB[4:8])
                for dc in range(8):
                    wb_ = wbr()
                    wm_ = wmg()
                    for i in range(4):
                        wload(wb_[:, :, i, :], W["w_branch"].ap()[li, i][:, dc * 128:(dc + 1) * 128].rearrange("(j p) n -> p j n", p=128), mstage)
                        wload(wm_[:, :, i, :], wview("w_in", li, OFF["mg"] + i * 1024 + dc * 128, 128), mstage)
                    for (t0, n) in TT:
                        a_ = acc()
                        for i in range(4):
                            pp = pbk()
                            for j in range(4):
                                I("pe", "matmul", out=pp[:, 0:n], lhsT=wb_[:, j, i, :], rhs=ysT[i][:, j, t0:t0 + n], start=(j == 0), stop=(j == 3))
                            pg = gbk()
                            for j in range(8):
                                I("pe", "matmul", out=pg[:, 0:n], lhsT=wm_[:, j, i, :], rhs=hT[:, j, t0:t0 + n], start=(j == 0), stop=(j == 7))
                            g_ = gs()
                            I("act", "activation", out=g_[:, 0:n], in_=pg[:, 0:n], func=AF.Sigmoid)
                            if i == 0:
                                I("dve", "tensor_tensor", out=a_[:, 0:n], in0=pp[:, 0:n], in1=g_[:, 0:n], op=ALU.mult)
                            else:
                                I("dve", "tensor_tensor", out=g_[:, 0:n], in0=pp[:, 0:n], in1=g_[:, 0:n], op=ALU.mult)
                                if i < 3:
                                    I("pool", "tensor_tensor", out=a_[:, 0:n], in0=a_[:, 0:n], in1=g_[:, 0:n], op=ALU.add)
                                else:
                                    I("pool", "tensor_tensor", out=mT[:, dc, t0:t0 + n], in0=a_[:, 0:n], in1=g_[:, 0:n], op=ALU.add)
                if b == 0 and li == 0:
                    dump("mT", mT[:, 0, :])
                k.barrier()
              if True:
                wout = k.sbs(s2, [128, 8, D], BF16, "wout")
                ostage = mkstage(s2, 256, nbuf=2)
                for hh in range(4):
                    wload(wout[:, :, hh * 256:(hh + 1) * 256], W["w_out"].ap()[li][:, hh * 256:(hh + 1) * 256].rearrange("(j p) n -> p j n", p=128), ostage)
                g1l = bcast_row(s2, modrow.ap()[li, b, 2048:3072], D, "g1l")
                g1c = bcast_row(s2, modrow.ap()[li, 2, 2048:3072], D, "g1c")
                lg = bcast_row(s2, W["ln1_g"].ap()[li], D, "lg")
                lb = bcast_row(s2, W["ln1_b"].ap()[li], D, "lb")
                tmp_pool = (k.sbs(s2, [128, 2, 6], F32, "stats"), k.sbs(s2, [128, 2], F32, "mv"), k.sbs(s2, [128, 1], F32, "rs"), None)
                xts = Rot([k.sbs(s2, [128, D], F32, "xt") for _ in range(2)])
                yts = Rot([k.sbs(s2, [128, D], F32, "yt") for _ in range(2)])
                ob = Rot(PB[0:4])
                for tk in range(18):
                    xt = xts()
                    yt = yts()
                    k.dma(out=xt[:], in_=xres.ap()[b, tk * 128:(tk + 1) * 128, :])
                    gb = g1c if tk < 2 else g1l
                    for hh in range(2):
                        ps = ob()
                        for j in range(8):
                            I("pe", "matmul", out=ps[:], lhsT=mT[:, j, tk * 128:(tk + 1) * 128], rhs=wout[:, j, hh * 512:(hh + 1) * 512], start=(j == 0), stop=(j == 7))
                        I("dve", "tensor_tensor", out=yt[:, hh * 512:(hh + 1) * 512], in0=ps[:], in1=gb[:, hh * 512:(hh + 1) * 512], op=ALU.mult)
                    I("dve", "scalar_tensor_tensor", out=xt[:], in0=xt[:], scalar=ALPHA, in1=yt[:], op0=ALU.mult, op1=ALU.add)
                    layer_norm_rows(xt, tmp_pool, lg, lb, 1e-5)
                    k.dma(out=xres.ap()[b, tk * 128:(tk + 1) * 128, :], in_=xt[:])
                    if b == 0 and li == 0 and tk == 2:
                        dump("x1", xt[:])
                k.barrier()
            k.barrier()

    def moe(li, b, last):
        with ExitStack() as st:
            modc, modc1p = load_modc(st, li)
            h2T = k.sbs(st, [128, 8, T], BF16, "h2T")
            G = k.sbs(st, [128, 18, NEXP], F32, "G")
            acc = k.sbs(st, [128, 18, D], F32, "acc")
            with ExitStack() as s2:
                tmp_pool = (k.sbs(s2, [128, 2, 6], F32, "stats"), k.sbs(s2, [128, 2], F32, "mv"), k.sbs(s2, [128, 1], F32, "rs"), k.sbs(s2, [128, D], F32, "xn"))
                xts = Rot([k.sbs(s2, [128, D], F32, "xt") for _ in range(2)])
                h2f = k.sbs(s2, [128, 8, 128], F32, "h2f")
                rw = k.sbs(s2, [128, 8, NEXP], F32, "rw")
                k.dma(out=rw[:], in_=W["router_w"].ap()[li].rearrange("(j p) n -> p j n", p=128))
                rb = bcast_row(s2, W["router_b"].ap()[li], NEXP, "rb")
                bd = k.sbs(s2, [NEXP, D], F32, "bd")
                k.dma(out=bd[:], in_=W["b_down"].ap()[li])
                lg_ = k.sbs(s2, [128, NEXP], F32, "lg_")
                m8 = k.sbs(s2, [128, 8], F32, "m8")
                nm = k.sbs(s2, [128, 1], F32, "nm")
                ex = k.sbs(s2, [128, NEXP], F32, "ex")
                mk = k.sbs(s2, [128, NEXP], F32, "mk")
                ssum = k.sbs(s2, [128, 1], F32, "ssum")
                GT = k.sbs(s2, [NEXP, 128], F32, "GT")
                for tk in range(18):
                    xt = xts()
                    k.dma(out=xt[:], in_=xres.ap()[b, tk * 128:(tk + 1) * 128, :])
                    r = 2 if tk < 2 else b
                    ln_mod_T(s2, xt, h2T, tk * 128, modc, modc1p, r, 32, 24, tmp_pool, extra_f32=h2f)
                    ps = PB[0]
                    for j in range(8):
                        I("pe", "matmul", out=ps[:, 0:NEXP], lhsT=h2f[:, j, :], rhs=rw[:, j, :], start=(j == 0), stop=(j == 7))
                    I("dve", "tensor_tensor", out=lg_[:], in0=ps[:, 0:NEXP], in1=rb[:], op=ALU.add)
                    I("dve", "max", out=m8[:], in_=lg_[:])
                    I("dve", "tensor_scalar", out=mk[:], in0=lg_[:], scalar1=m8[:, 3:4], scalar2=None, op0=ALU.is_ge)
                    I("dve", "tensor_scalar", out=nm[:], in0=m8[:, 0:1], scalar1=-1.0, scalar2=None, op0=ALU.mult)
                    I("act", "activation", out=ex[:], in_=lg_[:], func=AF.Exp, bias=nm[:, 0:1])
                    I("dve", "tensor_tensor", out=ex[:], in0=ex[:], in1=mk[:], op=ALU.mult)
                    I("dve", "reduce_sum", out=ssum[:], in_=ex[:], axis=AX.X)
                    I("dve", "reciprocal", out=ssum[:], in_=ssum[:])
                    I("dve", "tensor_scalar", out=G[:, tk, :], in0=ex[:], scalar1=ssum[:, 0:1], scalar2=None, op0=ALU.mult)
                    ps2 = PB[1]
                    I("pe", "transpose", out=ps2[0:NEXP, 0:128], in_=G[:, tk, :], identity=ident[:])
                    I("act", "activation", out=GT[:], in_=ps2[0:NEXP, 0:128], func=AF.Copy)
                    for hh in range(2):
                        ps3 = PB[2 + hh]
                        I("pe", "matmul", out=ps3[:], lhsT=GT[:], rhs=bd[:, hh * 512:(hh + 1) * 512], start=True, stop=True)
                        I("act", "activation", out=acc[:, tk, hh * 512:(hh + 1) * 512], in_=ps3[:], func=AF.Copy)
                if b == 0 and li == 0:
                    dump("G", G[:, 2, :])
                k.barrier()
            with ExitStack() as s2:
                actT = k.sbs(s2, [128, 8, T], BF16, "actT")
                bgc = k.sbs(s2, [128, 16, NEXP], F32, "bgc")
                bg17 = k.sbs(s2, [128, 8, NEXP], F32, "bg17")
                bu1 = k.sbs(s2, [128, 8, NEXP], F32, "bu1")
                sG = ExitStack()
                bgr = k.sbs(sG, [NEXP, 2048], F32, "bgr")
                k.dma(out=bgr[:], in_=W["b_gate_up"].ap()[li])
                for ch in range(16):
                    ps = PB[ch % 2]
                    I("pe", "transpose", out=ps[:, 0:NEXP], in_=bgr[0:NEXP, ch * 128:(ch + 1) * 128], identity=ident[0:NEXP, 0:NEXP])
                    I("act", "activation", out=bgc[:, ch, :], in_=ps[:, 0:NEXP], func=AF.Copy)
                I("dve", "tensor_scalar", out=bg17[:], in0=bgc[:, 0:8, :], scalar1=1.702, scalar2=None, op0=ALU.mult)
                I("dve", "tensor_scalar", out=bu1[:], in0=bgc[:, 8:16, :], scalar1=1.0, scalar2=None, op0=ALU.add)
                k.barrier()
                sG.close()
                stg = Rot([k.sbs(s2, [128, 8, 256], F32, "stg") for _ in range(2)])
                wgu_ = Rot([k.sbs(s2, [128, 8, 256], BF16, "wgu") for _ in range(2)])
                wdn_ = Rot([k.sbs(s2, [128, 8, 512], BF16, "wdn") for _ in range(2)])
                TTm = TT[1:] if last else TT
                tks = list(range(2, 18)) if last else list(range(18))
                gt_ = Rot([k.sbs(s2, [128, 512], F32, "gt") for _ in range(2)])
                sg_ = Rot([k.sbs(s2, [128, 512], F32, "sg") for _ in range(2)])
                ut_ = Rot([k.sbs(s2, [128, 512], F32, "ut") for _ in range(2)])
                gb = Rot(PB[0:3])
                ub = Rot(PB[3:6])
                db = Rot(PB[6:8])
                SIGC = 1.0 / (1.0 + math.exp(-1.702 * 7.0))
                pieces = []
                for e in range(NEXP):
                    for jp in range(8):
                        pieces.append(("gu", e, jp))
                    for q4 in range(2):
                        pieces.append(("dn", e, q4))

                def load_piece(pc):
                    kind, e, idx = pc
                    if kind == "gu":
                        s_ = stg()
                        w_ = wgu_()
                        k.dma(out=s_[:, :, 0:128], in_=W["w_gate_up"].ap()[li, e][:, idx * 128:(idx + 1) * 128].rearrange("(j p) n -> p j n", p=128))
                        k.dma(out=s_[:, :, 128:256], in_=W["w_gate_up"].ap()[li, e][:, D + idx * 128:D + (idx + 1) * 128].rearrange("(j p) n -> p j n", p=128))
                        I("pool", "tensor_copy", out=w_[:], in_=s_[:])
                    else:
                        w_ = wdn_()
                        for h2_ in range(2):
                            s_ = stg()
                            c0_ = idx * 512 + h2_ * 256
                            k.dma(out=s_[:], in_=W["w_down"].ap()[li, e][:, c0_:c0_ + 256].rearrange("(j p) n -> p j n", p=128))
                            I("pool", "tensor_copy", out=w_[:, :, h2_ * 256:(h2_ + 1) * 256], in_=s_[:])
                    return w_

                PF = 1
                loaded = {}
                for i in range(min(PF, len(pieces))):
                    loaded[i] = load_piece(pieces[i])
                for pi, (kind, e, idx) in enumerate(pieces):
                    if pi + PF < len(pieces):
                        loaded[pi + PF] = load_piece(pieces[pi + PF])
                    w_ = loaded.pop(pi)
                    if kind == "gu":
                        jp = idx
                        for (t0, n) in TTm:
                            pg = gb()
                            for j in range(8):
                                I("pe", "matmul", out=pg[:, 0:n], lhsT=w_[:, j, 0:128], rhs=h2T[:, j, t0:t0 + n], start=(j == 0), stop=(j == 7))
                            pu = ub()
                            for j in range(8):
                                I("pe", "matmul", out=pu[:, 0:n], lhsT=w_[:, j, 128:256], rhs=h2T[:, j, t0:t0 + n], start=(j == 0), stop=(j == 7))
                            gt = gt_()
                            sg = sg_()
                            ut = ut_()
                            I("act", "activation", out=sg[:, 0:n], in_=pg[:, 0:n], func=AF.Sigmoid, scale=1.702, bias=bg17[:, jp, e:e + 1])
                            I("dve", "tensor_scalar", out=gt[:, 0:n], in0=pg[:, 0:n], scalar1=bgc[:, jp, e:e + 1], scalar2=7.0, op0=ALU.add, op1=ALU.min)
                            I("dve", "tensor_scalar", out=ut[:, 0:n], in0=pu[:, 0:n], scalar1=bu1[:, jp, e:e + 1], scalar2=8.0, op0=ALU.add, op1=ALU.min)
                            I("dve", "scalar_tensor_tensor", out=gt[:, 0:n], in0=sg[:, 0:n], scalar=SIGC, in1=gt[:, 0:n], op0=ALU.min, op1=ALU.mult)
                            I("dve", "scalar_tensor_tensor", out=actT[:, jp, t0:t0 + n], in0=ut[:, 0:n], scalar=-6.0, in1=gt[:, 0:n], op0=ALU.max, op1=ALU.mult)
                    else:
                        q4 = idx
                        for tk in tks:
                            ps = db()
                            for j in range(8):
                                I("pe", "matmul", out=ps[:, :], lhsT=actT[:, j, tk * 128:(tk + 1) * 128], rhs=w_[:, j, :], start=(j == 0), stop=(j == 7))
                            I("dve", "scalar_tensor_tensor", out=acc[:, tk, q4 * 512:(q4 + 1) * 512], in0=ps[:, :], scalar=G[:, tk, e:e + 1],
                              in1=acc[:, tk, q4 * 512:(q4 + 1) * 512], op0=ALU.mult, op1=ALU.add)
                print("moe sbuf remaining", nc.sbuf_bytes_remaining, flush=True)
                k.barrier()
            if b == 0 and li == 0:
                dump("moe", acc[:, 2, :])
            with ExitStack() as s2:
                g2l = bcast_row(s2, modrow.ap()[li, b, 5120:6144], D, "g2l")
                g2c = bcast_row(s2, modrow.ap()[li, 2, 5120:6144], D, "g2c")
                lg = bcast_row(s2, W["ln2_g"].ap()[li], D, "lg2")
                lb = bcast_row(s2, W["ln2_b"].ap()[li], D, "lb2")
                tmp_pool = (k.sbs(s2, [128, 2, 6], F32, "stats"), k.sbs(s2, [128, 2], F32, "mv"), k.sbs(s2, [128, 1], F32, "rs"), None)
                xts = Rot([k.sbs(s2, [128, D], F32, "xt") for _ in range(2)])
                for tk in range(18):
                    if last and tk < 2:
                        continue
                    xt = xts()
                    k.dma(out=xt[:], in_=xres.ap()[b, tk * 128:(tk + 1) * 128, :])
                    gb_ = g2c if tk < 2 else g2l
                    I("pool", "tensor_tensor", out=acc[:, tk, :], in0=acc[:, tk, :], in1=gb_[:], op=ALU.mult)
                    I("dve", "scalar_tensor_tensor", out=xt[:], in0=xt[:], scalar=ALPHA, in1=acc[:, tk, :], op0=ALU.mult, op1=ALU.add)
                    layer_norm_rows(xt, tmp_pool, lg, lb, 1e-5)
                    if last:
                        k.dma(out=out.ap()[b, (tk - 2) * 128:(tk - 1) * 128, :], in_=xt[:])
                    else:
                        k.dma(out=xres.ap()[b, tk * 128:(tk + 1) * 128, :], in_=xt[:])
                k.barrier()
            k.barrier()

    for li in range(depth):
        adaln(li)
        for b in range(nb):
            mixer(li, b)
            if stop_after == "mixer":
                continue
            moe(li, b, last=(li == depth - 1 and stop_after is None))
    k.finish([out] + list(DBG.values()))
    print("instructions", k.n_inst, "waits", k.n_wait, flush=True)
    return nc


_NC_CACHE = {}


def kernel(**inputs):
    inputs = {k_: np.ascontiguousarray(np.asarray(v), dtype=np.float32) for k_, v in inputs.items()}
    if "nc" not in _NC_CACHE:
        _NC_CACHE["nc"] = build()
    nc = _NC_CACHE["nc"]
    consts = host_consts()
    in_maps = []
    for core in range(8):
        m = {}
        for name, arr in inputs.items():
            if name in ("x", "c", "ctx"):
                m[name] = np.ascontiguousarray(arr[2 * core:2 * core + 2])
            else:
                m[name] = arr
        m.update(consts)
        in_maps.append(m)
    res = run_bass_kernel_spmd(nc, in_maps, core_ids=list(range(8)))
    return np.concatenate([r["out"] for r in res.results], axis=0).astype(np.float32)
```

```python
import os
import numpy as np
import concourse.bass as bass
import concourse.mybir as mybir

F32 = mybir.dt.float32
BF16 = mybir.dt.bfloat16
F32R = mybir.dt.float32r
I32 = mybir.dt.int32
AF = mybir.ActivationFunctionType
ALU = mybir.AluOpType
AX = mybir.AxisListType

NDS = 24


class Trk:
    __slots__ = ("w", "r")

    def __init__(self):
        self.w = {}
        self.r = {}


class KB:
    def __init__(self, nc, same_engine_raw=True):
        self.nc = nc
        self.eng = {"pe": nc.tensor, "act": nc.scalar, "dve": nc.vector,
                    "pool": nc.gpsimd, "sp": nc.sync}
        self.sems = {}
        self.cnt = {}
        for k in ("pe", "act", "dve", "pool"):
            self.sems[k] = nc.alloc_semaphore("s_" + k)
            self.cnt[k] = 0
        self.dq = {}
        for q in ("sp", "act", "pool"):
            self.dq[q] = {"sems": [], "vals": [0] * NDS, "nxt": 0}
        for q in ("sp", "pool"):
            for i in range(NDS):
                key = ("d", q, i)
                self.sems[key] = nc.alloc_semaphore("d_%s_%d" % (q, i))
                self.dq[q]["sems"].append(key)
        self.seen = {k: {} for k in self.eng}
        self.trk = {}
        self.manual = set()
        self.psum_names = set()
        self.same_engine_raw = same_engine_raw and os.environ.get('SE_SYNC', '1') != '0'
        self.same_engine_war = os.environ.get('SE_SYNC', '1') == '1'
        self.n_inst = 0
        self.n_wait = 0
        self._uid = 0

    def _reg(self, t):
        self.trk[t.name] = Trk()
        return t

    def sb(self, shape, dtype=F32, name=None):
        self._uid += 1
        name = "%s_%d" % (name or "t", self._uid)
        return self._reg(self.nc.alloc_sbuf_tensor(name, list(shape), dtype))

    def ps(self, shape, dtype=F32, name=None):
        self._uid += 1
        name = "%s_%d" % (name or "p", self._uid)
        t = self._reg(self.nc.alloc_psum_tensor(name, list(shape), dtype))
        self.psum_names.add(t.name)
        return t

    def dram(self, name, shape, dtype=F32, kind="Internal", manual=False):
        t = self.nc.dram_tensor(name, list(shape), dtype, kind=kind)
        if manual:
            self.manual.add(t.name)
        else:
            self.trk[t.name] = Trk()
        return t

    def _key(self, x):
        if isinstance(x, (str, tuple)):
            if x not in self.trk:
                self.trk[x] = Trk()
            return x
        t = getattr(x, "tensor", None)
        if t is None:
            return None
        n = t.name
        if n in self.manual:
            return None
        return n if n in self.trk else None

    def _wait(self, ek, semkey, val):
        s = self.seen[ek]
        if s.get(semkey, 0) >= val:
            return
        s[semkey] = val
        self.eng[ek].wait_ge(self.sems[semkey], val)
        self.n_wait += 1

    def _deps(self, ek, reads, writes, is_dma=False):
        for x in reads:
            kx = self._key(x)
            tr = self.trk.get(kx)
            if tr is None:
                continue
            for sk, (v, wek) in tr.w.items():
                if sk == ek and (ek == "pe" or not self.same_engine_raw):
                    continue
                self._wait(ek, sk, v)
            if kx in self.psum_names:
                for sk, (v, rek) in tr.r.items():
                    if sk != ek:
                        self._wait(ek, sk, v)
        for x in writes:
            tr = self.trk.get(self._key(x))
            if tr is None:
                continue
            for sk, (v, wek) in tr.w.items():
                if sk == ek and (ek == "pe" or not self.same_engine_war):
                    continue
                if is_dma and isinstance(sk, tuple):
                    continue
                self._wait(ek, sk, v)
            for sk, (v, rek) in tr.r.items():
                if sk == ek and (ek == "pe" or not self.same_engine_war):
                    continue
                self._wait(ek, sk, v)

    def _mark(self, ek, tok, reads, writes, is_dma=False):
        sk, v = tok
        for x in reads:
            tr = self.trk.get(self._key(x))
            if tr is not None:
                tr.r[sk] = (v, ek)
        for x in writes:
            tr = self.trk.get(self._key(x))
            if tr is not None:
                if is_dma:
                    tr.w = {k: e for k, e in tr.w.items() if isinstance(k, tuple)}
                    tr.w[sk] = (v, ek)
                else:
                    tr.w = {sk: (v, ek)}
                tr.r = {}

    @staticmethod
    def _split(kwargs):
        reads, writes = [], []
        for k, v in kwargs.items():
            if v is None or isinstance(v, (int, float, bool, str)):
                continue
            if not hasattr(v, "tensor"):
                continue
            if k in ("out", "accum_out", "ap"):
                writes.append(v)
            else:
                reads.append(v)
        return reads, writes

    def I(self, ek, meth, extra_reads=(), extra_writes=(), **kwargs):
        reads, writes = self._split(kwargs)
        reads += list(extra_reads)
        writes += list(extra_writes)
        self._deps(ek, reads, writes)
        ins = getattr(self.eng[ek], meth)(**kwargs)
        self.cnt[ek] += 1
        ins.then_inc(self.sems[ek], 1)
        self._mark(ek, (ek, self.cnt[ek]), reads, writes)
        self.n_inst += 1
        return ins

    def dma(self, out, in_, q="sp", extra_reads=(), extra_writes=(), **kw):
        d = self.dq[q]
        i = d["nxt"]
        d["nxt"] = (i + 1) % NDS
        key = d["sems"][i]
        if d["vals"][i] > 0:
            self._wait(q, key, d["vals"][i])
        reads, writes = [in_], [out]
        reads += list(extra_reads)
        writes += list(extra_writes)
        self._deps(q, reads, writes, is_dma=True)
        ins = self.eng[q].dma_start(out=out, in_=in_, **kw)
        d["vals"][i] += 16
        ins.then_inc(self.sems[key], 16)
        self._mark(q, (key, d["vals"][i]), reads, writes, is_dma=True)
        self.n_inst += 1
        return ins

    def finish(self, outs):
        for t in outs:
            tr = self.trk.get(self._key(t))
            if tr is not None:
                for sk, (v, ek) in tr.w.items():
                    self._wait("sp", sk, v)
        for q, d in self.dq.items():
            for i, key in enumerate(d["sems"]):
                if d["vals"][i] > 0:
                    self._wait("sp", key, d["vals"][i])

    def barrier(self):
        toks = [(ek, self.cnt[ek]) for ek in ("pe", "act", "dve", "pool") if self.cnt[ek] > 0]
        for q, d in self.dq.items():
            for i, key in enumerate(d["sems"]):
                if d["vals"][i] > 0:
                    toks.append((key, d["vals"][i]))
        for ek in ("pe", "act", "dve", "pool", "sp"):
            for sk, v in toks:
                if sk == ek:
                    continue
                self._wait(ek, sk, v)

    def sbs(self, stack, shape, dtype=F32, name=None):
        self._uid += 1
        name = "%s_%d" % (name or "s", self._uid)
        t = stack.enter_context(self.nc.sbuf_tensor(name, list(shape), dtype))
        self.trk[t.name] = Trk()
        return t


from contextlib import ExitStack
import math, os
from concourse.bass_utils import run_bass_kernel_spmd

D = 1024
T = 2304
NCTX = 256
NLAT = 2048
TT = [(0, 256), (256, 512), (768, 512), (1280, 512), (1792, 512)]
OFF = dict(rx=0, rg=512, q=1024, k=1536, v=2048, pu=2560, z=3072, xbc=3584, dt=4352, mg=4368)
ALPHA = 8.0 ** 0.25
NEXP = 32

WSPEC = [
    ("w_ada", [4, 1024, 6144]), ("b_ada", [4, 6144]), ("w_in", [4, 1024, 8464]),
    ("rnn_conv_w", [4, 4, 512]), ("rnn_conv_b", [4, 512]), ("rnn_wa", [4, 2, 8, 64, 64]),
    ("rnn_ba", [4, 2, 512]), ("rnn_wx", [4, 2, 8, 64, 64]), ("rnn_bx", [4, 2, 512]),
    ("rnn_lam", [4, 2, 512]), ("att_lambda", [4, 4, 64]), ("att_subln", [4, 128]),
    ("pool_w", [4, 4, 128, 128]), ("pool_b", [4, 512]), ("pool_scale", [4, 512]),
    ("ssm_conv_w", [4, 4, 768]), ("ssm_conv_b", [4, 768]), ("ssm_dt_bias", [4, 2, 8]),
    ("ssm_a_log", [4, 2, 8]), ("ssm_d", [4, 2, 8]), ("ssm_norm", [4, 512]),
    ("w_branch", [4, 4, 512, 1024]), ("w_out", [4, 1024, 1024]),
    ("ln1_g", [4, 1024]), ("ln1_b", [4, 1024]), ("ln2_g", [4, 1024]), ("ln2_b", [4, 1024]),
    ("router_w", [4, 1024, 32]), ("router_b", [4, 32]),
    ("w_gate_up", [4, 32, 1024, 2048]), ("b_gate_up", [4, 32, 2048]),
    ("w_down", [4, 32, 1024, 1024]), ("b_down", [4, 32, 1024]),
]
CSPEC = [("k_ident", [128, 128]), ("k_cs", [2, 128, 2048]), ("k_perm", [128, 128]),
         ("k_mask", [2, 128, 896]), ("k_esel", [16, 2048])]


def host_consts():
    ident = np.eye(128, dtype=np.float32)
    t = np.arange(NLAT)
    row = (t // 64).astype(np.float32)
    col = (t % 64).astype(np.float32)
    inv = (10000.0 ** (-np.arange(16, dtype=np.float32) / 16)).astype(np.float32)
    cs = np.zeros((2, 128, NLAT), np.float32)
    perm = np.zeros((128, 128), np.float32)
    for p in range(128):
        d = p % 64
        axis, half, fr = d // 32, (d % 32) // 16, d % 16
        pos = row if axis == 0 else col
        ang = (pos * inv[fr]).astype(np.float32)
        cs[0, p] = np.cos(ang)
        cs[1, p] = np.sin(ang) * (-1.0 if half == 0 else 1.0)
        partner = p + 16 if half == 0 else p - 16
        perm[partner, p] = 1.0
    cc = np.arange(-384, 512)[None, :]
    sp = np.arange(128)[:, None]
    mask = np.stack([(cc >= sp), (cc <= sp)]).astype(np.float32)
    esel = np.zeros((16, 16 * 128), np.float32)
    for j in range(16):
        esel[j, j * 128:(j + 1) * 128] = 1.0
    return dict(k_ident=ident, k_cs=cs, k_perm=perm, k_mask=mask, k_esel=esel)


def build(depth=4, dbg=(), nb=2, stop_after=None, stages=("rglru", "pool", "attn", "ssd", "merge"), wover=None, cut=99):
    nc = bass.Bass("TRN2", target_bir_lowering=False)
    k = KB(nc)
    I = k.I

    def ext(name, shape, kind="ExternalInput", dtype=F32):
        t = nc.dram_tensor(name, list(shape), dtype, kind=kind)
        k.trk[t.name] = Trk()
        return t

    x_in = ext("x", [2, NLAT, D])
    c_in = ext("c", [2, D])
    ctx_in = ext("ctx", [2, NCTX, D])
    cctx_in = ext("c_ctx", [D])
    W = {n: ext(n, (wover or {}).get(n, s)) for n, s in WSPEC}
    C = {n: ext(n, s) for n, s in CSPEC}
    out = ext("out", [2, NLAT, D], kind="ExternalOutput")
    xres = k.dram("xres", [2, T, D])
    modrow = k.dram("modrow", [4, 3, 6144])
    ysd = k.dram("ysd", [2, 4, 128, 4, T], BF16)
    DBG = {}
    for name, shape, dt_ in dbg:
        DBG[name] = ext("dbg_" + name, shape, kind="ExternalOutput", dtype=dt_)

    PB = [k.ps([128, 512], F32, "bank") for _ in range(8)]

    class Rot:
        def __init__(self, lst):
            self.l = lst
            self.i = 0

        def __call__(self):
            r = self.l[self.i % len(self.l)]
            self.i += 1
            return r

    ident = k.sb([128, 128], F32, "ident")
    k.dma(out=ident[:], in_=C["k_ident"].ap())
    ones = k.sb([128, 128], F32, "ones")
    I("dve", "memset", ap=ones[:], constant=1.0)
    onesb = k.sb([128, 128], BF16, "onesb")
    I("dve", "memset", ap=onesb[:], constant=1.0)
    cT = k.sb([128, 3, 8], F32, "cT")
    k.dma(out=cT[:, 0:2, :], in_=c_in.ap().rearrange("b (p j) -> p b j", j=8))
    k.dma(out=cT[:, 2, :], in_=cctx_in.ap().rearrange("(p j) -> p j", j=8))
    sT = k.sb([128, 3, 8], F32, "sT")
    I("act", "activation", out=sT[:], in_=cT[:], func=AF.Silu)
    for b in range(nb):
        k.dma(out=xres.ap()[b, 0:NCTX, :], in_=ctx_in.ap()[b])
        k.dma(out=xres.ap()[b, NCTX:T, :], in_=x_in.ap()[b])

    def dump(name, src_ap):
        if name in DBG:
            k.dma(out=DBG[name].ap(), in_=src_ap)

    def colvec(st, src1d, nch, name="cv"):
        t = k.sbs(st, [128, nch], F32, name)
        k.dma(out=t[:], in_=src1d.rearrange("(j p) -> p j", p=128), allow_slow_non_contiguous=True)
        return t

    def rstd_of(st_tile_var, eps, tmp):
        I("dve", "tensor_scalar", out=tmp, in0=st_tile_var, scalar1=float(eps), scalar2=None, op0=ALU.add)
        I("act", "activation", out=tmp, in_=tmp, func=AF.Sqrt)
        I("dve", "reciprocal", out=tmp, in_=tmp)

    def adaln(li):
        with ExitStack() as st:
            mrow = k.sbs(st, [3, 6144], F32, "mrow")
            brow = k.sbs(st, [3, 6144], F32, "brow")
            k.dma(out=brow[:], in_=W["b_ada"].ap()[li].partition_broadcast(3))
            wps = Rot([k.sbs(st, [128, 8, 512], F32, "wada") for _ in range(2)])
            wv = W["w_ada"].ap()[li].rearrange("(p j) n -> p j n", j=8)
            pr = Rot(PB[0:2])
            for n in range(12):
                wp = wps()
                k.dma(out=wp[:], in_=wv[:, :, n * 512:(n + 1) * 512])
                ps = pr()
                for j in range(8):
                    I("pe", "matmul", out=ps[0:3, :], lhsT=sT[:, :, j], rhs=wp[:, j, :], start=(j == 0), stop=(j == 7))
                I("dve", "tensor_tensor", out=mrow[:, n * 512:(n + 1) * 512], in0=ps[0:3, :], in1=brow[:, n * 512:(n + 1) * 512], op=ALU.add)
            k.dma(out=modrow.ap()[li], in_=mrow[:])
            k.barrier()

    def ln_mod_T(st, xt, dstT, col0, modc, modc1p, r, sc0, sh0, tmp_pool, extra_f32=None):
        stats, mv, rs, xn = tmp_pool
        for h in range(2):
            I("dve", "bn_stats", out=stats[:, h, :], in_=xt[:, h * 512:(h + 1) * 512])
        I("dve", "bn_aggr", out=mv[:], in_=stats[:])
        rstd_of(mv[:, 1:2], 1e-6, rs[:])
        I("dve", "tensor_scalar", out=xn[:], in0=xt[:], scalar1=mv[:, 0:1], scalar2=rs[:, 0:1], op0=ALU.subtract, op1=ALU.mult)
        for h in range(2):
            ps = PB[6 + h]
            for jj in range(4):
                j = h * 4 + jj
                I("pe", "transpose", out=ps[:, jj * 128:(jj + 1) * 128], in_=xn[:, j * 128:(j + 1) * 128], identity=ident[:])
            for jj in range(4):
                j = h * 4 + jj
                I("act", "activation", out=dstT[:, j, col0:col0 + 128], in_=ps[:, jj * 128:(jj + 1) * 128], func=AF.Identity,
                  scale=modc1p[:, sc0 + j, r:r + 1], bias=modc[:, sh0 + j, r:r + 1])
                if extra_f32 is not None:
                    I("act", "activation", out=extra_f32[:, j, :], in_=ps[:, jj * 128:(jj + 1) * 128], func=AF.Identity,
                      scale=modc1p[:, sc0 + j, r:r + 1], bias=modc[:, sh0 + j, r:r + 1])

    def load_modc(st, li):
        modc = k.sbs(st, [128, 48, 3], F32, "modc")
        modc1p = k.sbs(st, [128, 48, 3], F32, "modc1p")
        sM = ExitStack()
        mr = k.sbs(sM, [3, 6144], F32, "mr")
        k.dma(out=mr[:], in_=modrow.ap()[li])
        ps = PB[0]
        for j in range(48):
            I("pe", "transpose", out=ps[:, j * 3:(j + 1) * 3], in_=mr[0:3, j * 128:(j + 1) * 128], identity=ident[0:3, 0:3])
        I("dve", "tensor_copy", out=modc[:].rearrange("p a b -> p (a b)"), in_=ps[:, 0:144])
        I("dve", "tensor_scalar", out=modc1p[:].rearrange("p a b -> p (a b)"), in0=ps[:, 0:144], scalar1=1.0, scalar2=None, op0=ALU.add)
        k.barrier()
        sM.close()
        return modc, modc1p

    def mkstage(st, ncols, nbuf=2, kc=8):
        return Rot([k.sbs(st, [128, kc, ncols], F32, "wstg") for _ in range(nbuf)])

    def wload(dst, src, stage):
        s_ = stage()
        kc_, n_ = dst.shape[1], dst.shape[2]
        k.dma(out=s_[:, 0:kc_, 0:n_], in_=src)
        I("pool", "tensor_copy", out=dst, in_=s_[:, 0:kc_, 0:n_])

    def wview(name, li, c0, n):
        return W[name].ap()[li][:, c0:c0 + n].rearrange("(j p) n -> p j n", p=128)

    def proj(st, hT, li, c0, ncols, evac, wpool, tts=TT, banks=None):
        wt = wpool()
        wload(wt[:, :, 0:ncols], wview("w_in", li, c0, ncols), wpool.stage)
        banks = banks or Rot(PB[0:4])
        for (t0, n) in tts:
            ps = banks()
            for j in range(8):
                I("pe", "matmul", out=ps[0:ncols, 0:n], lhsT=wt[:, j, 0:ncols], rhs=hT[:, j, t0:t0 + n], start=(j == 0), stop=(j == 7))
            evac(ps, t0, n)

    SEG = [(0, NCTX), (NCTX, NLAT)]

    def conv_seg(xp, u, wc, bc):
        for si, (s0, L) in enumerate(SEG):
            p0 = 2 + s0 + 4 * si
            I("dve", "tensor_scalar", out=u[:, s0:s0 + L], in0=xp[:, p0 - 2:p0 - 2 + L], scalar1=wc[:, 0:1], scalar2=bc, op0=ALU.mult, op1=ALU.add)
            for kk in range(1, 4):
                I("dve", "scalar_tensor_tensor", out=u[:, s0:s0 + L], in0=xp[:, p0 - 2 + kk:p0 - 2 + kk + L], scalar=wc[:, kk:kk + 1],
                  in1=u[:, s0:s0 + L], op0=ALU.mult, op1=ALU.add)

    def pad_zero(xp):
        for (a, b_) in [(0, 2), (258, 262), (2310, 2312)]:
            I("pool", "memset", ap=xp[:, a:b_], constant=0.0)

    def xp_cols(t0, n):
        return (t0 + 2) if t0 < NCTX else (t0 + 6)

    def branch_rglru(st0, hT, li, b):
        with ExitStack() as st:
            wpool = Rot([k.sbs(st, [128, 8, 128], BF16, "wq") for _ in range(2)])
            wpool.stage = mkstage(st, 128)
            cw = k.sbs(st, [128, 4, 4], F32, "rcw")
            for kk in range(4):
                k.dma(out=cw[:, :, kk], in_=W["rnn_conv_w"].ap()[li, kk].rearrange("(j p) -> p j", p=128), allow_slow_non_contiguous=True)
            cb = colvec(st, W["rnn_conv_b"].ap()[li], 4, "rcb")
            ba = [colvec(st, W["rnn_ba"].ap()[li, d], 4, "rba") for d in range(2)]
            bx = [colvec(st, W["rnn_bx"].ap()[li, d], 4, "rbx") for d in range(2)]
            lam = [colvec(st, W["rnn_lam"].ap()[li, d], 4, "rlam") for d in range(2)]
            cpl = [k.sbs(st, [128, 4], F32, "cpl") for d in range(2)]
            cpl2 = [k.sbs(st, [128, 4], F32, "cpl2") for d in range(2)]
            tl = k.sbs(st, [128, 4], F32, "tl")
            for d in range(2):
                I("act", "activation", out=tl[:], in_=lam[d][:], func=AF.Abs)
                I("act", "activation", out=tl[:], in_=tl[:], func=AF.Exp, scale=-1.0)
                I("act", "activation", out=tl[:], in_=tl[:], func=AF.Ln, bias=1.0)
                I("dve", "tensor_scalar", out=cpl[d][:], in0=lam[d][:], scalar1=-1.0, scalar2=0.0, op0=ALU.mult, op1=ALU.max)
                I("dve", "tensor_tensor", out=cpl[d][:], in0=cpl[d][:], in1=tl[:], op=ALU.add)
                I("dve", "tensor_scalar", out=cpl2[d][:], in0=cpl[d][:], scalar1=-16.0, scalar2=None, op0=ALU.mult)
                I("dve", "tensor_scalar", out=cpl[d][:], in0=cpl[d][:], scalar1=-8.0, scalar2=None, op0=ALU.mult)
            wbd = [[k.sbs(st, [128, 128], F32, "wbd") for _ in range(2)] for _ in range(2)]
            xp = k.sbs(st, [128, T + 8], F32, "rxp")
            u = k.sbs(st, [128, T], F32, "ru")
            av = k.sbs(st, [128, T], F32, "ra")
            bv = k.sbs(st, [128, T], F32, "rb")
            hf = k.sbs(st, [128, T], F32, "rhf")
            it = Rot([k.sbs(st, [128, 512], F32, "rit") for _ in range(2)])
            yo = k.sbs(st, [128, T], BF16, "ryo")
            pad_zero(xp)
            for d in range(2):
                for w_ in range(2):
                    I("pool", "memset", ap=wbd[d][w_][:], constant=0.0)
            for fc in range(4):
                def ev_rx(ps, t0, n):
                    pc = xp_cols(t0, n)
                    I("act", "activation", out=xp[:, pc:pc + n], in_=ps[:, 0:n], func=AF.Copy)
                proj(st, hT, li, OFF["rx"] + fc * 128, 128, ev_rx, wpool)
                conv_seg(xp, u, cw[:, fc, :], cb[:, fc:fc + 1])
                for d in range(2):
                    for w_, nm in enumerate(("rnn_wa", "rnn_wx")):
                        for blk in range(2):
                            k.dma(out=wbd[d][w_][blk * 64:(blk + 1) * 64, blk * 64:(blk + 1) * 64], in_=W[nm].ap()[li, d, fc * 2 + blk])
                for d in range(2):
                    banks = Rot(PB[4:6])
                    for (t0, n) in TT:
                        psa = banks()
                        I("pe", "matmul", out=psa[:, 0:n], lhsT=wbd[d][0][:], rhs=u[:, t0:t0 + n], start=True, stop=True)
                        rt = it()
                        I("act", "activation", out=rt[:, 0:n], in_=psa[:, 0:n], func=AF.Sigmoid, bias=ba[d][:, fc:fc + 1])
                        I("act", "activation", out=av[:, t0:t0 + n], in_=rt[:, 0:n], func=AF.Exp, scale=cpl[d][:, fc:fc + 1])
                        I("act", "activation", out=bv[:, t0:t0 + n], in_=rt[:, 0:n], func=AF.Exp, scale=cpl2[d][:, fc:fc + 1])
                        psx = banks()
                        I("pe", "matmul", out=psx[:, 0:n], lhsT=wbd[d][1][:], rhs=u[:, t0:t0 + n], start=True, stop=True)
                        it2 = it()
                        I("act", "activation", out=it2[:, 0:n], in_=psx[:, 0:n], func=AF.Sigmoid, bias=bx[d][:, fc:fc + 1])
                        I("pool", "tensor_tensor", out=it2[:, 0:n], in0=it2[:, 0:n], in1=u[:, t0:t0 + n], op=ALU.mult)
                        I("dve", "tensor_scalar", out=bv[:, t0:t0 + n], in0=bv[:, t0:t0 + n], scalar1=-1.0, scalar2=1.0, op0=ALU.mult, op1=ALU.add)
                        I("act", "activation", out=bv[:, t0:t0 + n], in_=bv[:, t0:t0 + n], func=AF.Sqrt)
                        I("dve", "tensor_tensor", out=bv[:, t0:t0 + n], in0=bv[:, t0:t0 + n], in1=it2[:, 0:n], op=ALU.mult)
                    if d == 0:
                        I("dve", "tensor_tensor_scan", out=hf[:, :], data0=av[:, :], data1=bv[:, :], initial=0.0, op0=ALU.mult, op1=ALU.add)
                    else:
                        hb = xp
                        I("dve", "tensor_tensor_scan", out=hb[:, 255::-1], data0=av[:, 255::-1], data1=bv[:, 255::-1], initial=0.0, op0=ALU.mult, op1=ALU.add)
                        I("dve", "tensor_tensor_scan", out=hb[:, T - 1:255:-1], data0=av[:, T - 1:255:-1], data1=bv[:, T - 1:255:-1],
                          initial=hb[:, 0:1], op0=ALU.mult, op1=ALU.add)
                        I("dve", "tensor_tensor", out=hf[:, :], in0=hf[:, :], in1=hb[:, 0:T], op=ALU.add)
                def ev_rg(ps, t0, n):
                    g = it()
                    g2 = it()
                    I("act", "activation", out=g[:, 0:n], in_=ps[:, 0:n], func=AF.Square)
                    I("dve", "tensor_scalar", out=g[:, 0:n], in0=g[:, 0:n], scalar1=0.044715, scalar2=1.0, op0=ALU.mult, op1=ALU.add)
                    I("dve", "tensor_tensor", out=g[:, 0:n], in0=g[:, 0:n], in1=ps[:, 0:n], op=ALU.mult)
                    I("act", "activation", out=g[:, 0:n], in_=g[:, 0:n], func=AF.Sigmoid, scale=2.0 * math.sqrt(2.0 / math.pi))
                    I("dve", "tensor_tensor", out=g2[:, 0:n], in0=g[:, 0:n], in1=ps[:, 0:n], op=ALU.mult)
                    I("pool", "tensor_tensor", out=yo[:, t0:t0 + n], in0=g2[:, 0:n], in1=hf[:, t0:t0 + n], op=ALU.mult)
                proj(st, hT, li, OFF["rg"] + fc * 128, 128, ev_rg, wpool)
                k.dma(out=ysd.ap()[b, 0, :, fc, :], in_=yo[:])
                if fc == 0:
                    dump("rnn0", yo[:])
                pad_zero(xp)
            k.barrier()

    def branch_pool(st0, hT, li, b):
        with ExitStack() as st:
            wpool = Rot([k.sbs(st, [128, 8, 128], BF16, "wq") for _ in range(2)])
            wpool.stage = mkstage(st, 128)
            pb = colvec(st, W["pool_b"].ap()[li], 4, "pb")
            psc = colvec(st, W["pool_scale"].ap()[li], 4, "psc")
            I("dve", "tensor_tensor", out=pb[:], in0=pb[:], in1=psc[:], op=ALU.mult)
            pw = k.sbs(st, [128, 4, 128], BF16, "pw")
            wload(pw[:], W["pool_w"].ap()[li].rearrange("g c e -> c g e"), wpool.stage)
            PADL = 16
            segoff = [PADL, PADL + NCTX + 32]
            WP = NCTX + NLAT + 64 + 16
            xp = k.sbs(st, [128, WP], F32, "pxp")
            s1 = k.sbs(st, [128, WP], F32, "ps1")
            s2 = k.sbs(st, [128, WP], F32, "ps2")
            mix = k.sbs(st, [128, T], BF16, "pmix")
            yo = k.sbs(st, [128, T], BF16, "pyo")
            I("pool", "memset", ap=xp[:], constant=0.0)
            for g in range(4):
                win = (2, 4, 8, 16)[g]

                def ev(ps, t0, n):
                    si = 0 if t0 < NCTX else 1
                    pc = segoff[si] + (t0 - SEG[si][0])
                    I("act", "activation", out=xp[:, pc:pc + n], in_=ps[:, 0:n], func=AF.Copy)
                proj(st, hT, li, OFF["pu"] + g * 128, 128, ev, wpool)
                for si, (s0, L) in enumerate(SEG):
                    o = segoff[si]
                    lo, hi = o - 12, o + L + 12
                    I("dve", "tensor_tensor", out=s1[:, lo:hi], in0=xp[:, lo:hi], in1=xp[:, lo - 1:hi - 1], op=ALU.add)
                    cur, oth = s1, s2
                    sh = 1
                    while sh * 2 < win:
                        lo, hi = lo + sh, hi - sh
                        I("dve", "tensor_tensor", out=oth[:, lo:hi], in0=cur[:, lo + sh:hi + sh], in1=cur[:, lo - sh:hi - sh], op=ALU.add)
                        cur, oth = oth, cur
                        sh *= 2
                    res = oth
                    I("dve", "tensor_scalar", out=res[:, o:o + L], in0=cur[:, o:o + L], scalar1=1.0 / win, scalar2=None, op0=ALU.mult)
                    for tpos in range(L):
                        lo_ = max(tpos - win // 2, 0)
                        hi_ = min(tpos + win - 1 - win // 2, L - 1)
                        cnt = hi_ - lo_ + 1
                        if cnt != win:
                            I("dve", "tensor_scalar", out=res[:, o + tpos:o + tpos + 1], in0=cur[:, o + tpos:o + tpos + 1], scalar1=1.0 / cnt, scalar2=None, op0=ALU.mult)
                        if tpos == win and L > 4 * win:
                            pass
                    I("dve", "tensor_tensor", out=mix[:, s0:s0 + L], in0=res[:, o:o + L], in1=xp[:, o:o + L], op=ALU.subtract)
                banks = Rot(PB[4:6])
                for (t0, n) in TT:
                    ps = banks()
                    I("pe", "matmul", out=ps[:, 0:n], lhsT=pw[:, g, :], rhs=mix[:, t0:t0 + n], start=True, stop=True)
                    I("act", "activation", out=yo[:, t0:t0 + n], in_=ps[:, 0:n], func=AF.Identity, scale=psc[:, g:g + 1], bias=pb[:, g:g + 1])
                k.dma(out=ysd.ap()[b, 2, :, g, :], in_=yo[:])
                if g == 3:
                    dump("pool3", yo[:])
            k.barrier()

    def branch_attn(st0, hT, li, b):
        lam_init = 0.8 - 0.6 * math.exp(-0.3 * li)
        with ExitStack() as st:
            wpool = Rot([k.sbs(st, [128, 8, 512], BF16, "wq") for _ in range(2)])
            wpool.stage = mkstage(st, 512, nbuf=1)
            cosT = k.sbs(st, [128, NLAT], F32, "cos")
            sinT = k.sbs(st, [128, NLAT], F32, "sin")
            k.dma(out=cosT[:], in_=C["k_cs"].ap()[0])
            k.dma(out=sinT[:], in_=C["k_cs"].ap()[1])
            perm = k.sbs(st, [128, 128], BF16, "perm")
            wload(perm[:].rearrange("p (a n) -> p a n", a=1), C["k_perm"].ap().rearrange("p (a n) -> p a n", a=1), wpool.stage)
            lv = k.sbs(st, [128, 256], F32, "lv")
            k.dma(out=lv[:], in_=W["att_lambda"].ap()[li].rearrange("a b -> (a b)").partition_broadcast(128))
            l2 = k.sbs(st, [128, 2], F32, "l2")
            lt = k.sbs(st, [128, 64], F32, "lt")
            for i2 in range(2):
                I("dve", "tensor_tensor", out=lt[:], in0=lv[:, i2 * 128:i2 * 128 + 64], in1=lv[:, i2 * 128 + 64:i2 * 128 + 128], op=ALU.mult)
                I("dve", "reduce_sum", out=l2[:, i2:i2 + 1], in_=lt[:], axis=AX.X)
            I("act", "activation", out=l2[:], in_=l2[:], func=AF.Exp)
            nlam = k.sbs(st, [128, 1], F32, "nlam")
            I("dve", "tensor_tensor", out=nlam[:], in0=l2[:, 1:2], in1=l2[:, 0:1], op=ALU.subtract)
            I("dve", "tensor_scalar", out=nlam[:], in0=nlam[:], scalar1=-lam_init, scalar2=None, op0=ALU.add)
            sw = k.sbs(st, [128, 1], F32, "sw")
            k.dma(out=sw[:], in_=W["att_subln"].ap()[li].rearrange("(p o) -> p o", o=1))
            I("dve", "tensor_scalar", out=sw[:], in0=sw[:], scalar1=1.0 - lam_init, scalar2=None, op0=ALU.mult)
            vt = k.sbs(st, [128, 18, 512], BF16, "vt")
            wv_ = wpool()
            wload(wv_[:], wview("w_in", li, OFF["v"], 512), wpool.stage)
            vb = Rot(PB[0:2])
            for tk in range(18):
                ps = vb()
                for j in range(8):
                    I("pe", "matmul", out=ps[:], lhsT=hT[:, j, tk * 128:(tk + 1) * 128], rhs=wv_[:, j, :], start=(j == 0), stop=(j == 7))
                I("act", "activation", out=vt[:, tk, :], in_=ps[:], func=AF.Copy)
            if cut <= 1:
                k.barrier()
                return
            qT = k.sbs(st, [128, T], BF16, "qT")
            kT = k.sbs(st, [128, T], BF16, "kT")
            tmpa = Rot([k.sbs(st, [128, 512], F32, "ta") for _ in range(3)])
            pT = Rot([k.sbs(st, [128, 512], BF16, "pT") for _ in range(4)])
            osb = k.sbs(st, [128, 512], F32, "osb")
            osq = k.sbs(st, [128, 512], F32, "osq")
            yo = k.sbs(st, [128, T], BF16, "ayo")
            for hd in range(4):
                wqk = wpool()
                wload(wqk[:, :, 0:128], wview("w_in", li, OFF["q"] + hd * 128, 128), wpool.stage)
                wload(wqk[:, :, 128:256], wview("w_in", li, OFF["k"] + hd * 128, 128), wpool.stage)
                banks = Rot(PB[0:2])
                for which, dst in ((0, qT), (1, kT)):
                    for (t0, n) in TT:
                        ps = banks()
                        for j in range(8):
                            I("pe", "matmul", out=ps[:, 0:n], lhsT=wqk[:, j, which * 128:(which + 1) * 128], rhs=hT[:, j, t0:t0 + n], start=(j == 0), stop=(j == 7))
                        if t0 < NCTX or os.environ.get("ROPE_OFF") == "1":
                            I("act", "activation", out=dst[:, t0:t0 + n], in_=ps[:, 0:n], func=AF.Copy)
                        else:
                            RS = int(os.environ.get("ROPE_STEP", "9"))
                            xb = pT()
                            I("act", "activation", out=xb[:, 0:n], in_=ps[:, 0:n], func=AF.Copy)
                            if RS <= 1:
                                I("act", "activation", out=dst[:, t0:t0 + n], in_=ps[:, 0:n], func=AF.Copy)
                                continue
                            ps2 = banks()
                            I("pe", "matmul", out=ps2[:, 0:n], lhsT=perm[:], rhs=xb[:, 0:n], start=True, stop=True)
                            if RS <= 2:
                                I("act", "activation", out=dst[:, t0:t0 + n], in_=ps2[:, 0:n], func=AF.Copy)
                                continue
                            ta = tmpa()
                            V = os.environ.get("RV", "0")
                            if V == "1":
                                I("dve", "tensor_tensor", out=ta[:, 0:n], in0=xb[:, 0:n], in1=cosT[:, t0 - NCTX:t0 - NCTX + n], op=ALU.mult)
                            elif V == "2":
                                I("dve", "tensor_copy", out=ta[:, 0:n], in_=cosT[:, t0 - NCTX:t0 - NCTX + n])
                            elif V == "3":
                                I("dve", "tensor_copy", out=ta[:, 0:n], in_=ps[:, 0:n])
                            else:
                                I("dve", "tensor_tensor", out=ta[:, 0:n], in0=ps[:, 0:n], in1=cosT[:, t0 - NCTX:t0 - NCTX + n], op=ALU.mult)
                            if RS <= 3:
                                I("act", "activation", out=dst[:, t0:t0 + n], in_=ta[:, 0:n], func=AF.Copy)
                                continue
                            tb = tmpa()
                            I("dve", "tensor_tensor", out=tb[:, 0:n], in0=ps2[:, 0:n], in1=sinT[:, t0 - NCTX:t0 - NCTX + n], op=ALU.mult)
                            I(os.environ.get("ROPE_ENG", "pool"), "tensor_tensor", out=dst[:, t0:t0 + n], in0=ta[:, 0:n], in1=tb[:, 0:n], op=ALU.add)
                if cut <= 2:
                    continue
                for (t0, n) in TT:
                    if cut <= 3 and t0 > 0:
                        continue
                    kchunks = [0, 1] if t0 < NCTX else list(range(18))
                    Ob = [PB[2], PB[3]]
                    Zb = [PB[4], PB[5]]
                    sb_ = Rot([PB[6], PB[7], PB[0], PB[1]])
                    steps = [(ci, kc, cm) for ci, kc in enumerate(kchunks) for cm in range(2)]

                    def s_mm(step):
                        ci, kc, cm = step
                        ps = sb_()
                        I("pe", "matmul", out=ps[:, 0:n], lhsT=kT[cm * 64:(cm + 1) * 64, kc * 128:(kc + 1) * 128],
                          rhs=qT[cm * 64:(cm + 1) * 64, t0:t0 + n], start=True, stop=True)
                        return ps
                    LA = 2
                    psq = [s_mm(steps[i_]) for i_ in range(min(LA, len(steps)))]
                    for si_, (ci, kc, cm) in enumerate(steps):
                        ps = psq.pop(0)
                        pt = pT()
                        I("act", "activation", out=pt[:, 0:n], in_=ps[:, 0:n], func=AF.Exp, scale=0.125)
                        if si_ + LA < len(steps):
                            psq.append(s_mm(steps[si_ + LA]))
                        I("pe", "matmul", out=Ob[cm][:, 0:n], lhsT=vt[:, kc, hd * 128:(hd + 1) * 128], rhs=pt[:, 0:n],
                          start=(ci == 0), stop=(ci == len(kchunks) - 1))
                        I("pe", "matmul", out=Zb[cm][:, 0:n], lhsT=onesb[:], rhs=pt[:, 0:n],
                          start=(ci == 0), stop=(ci == len(kchunks) - 1))
                    r0 = tmpa()
                    I("dve", "reciprocal", out=r0[:, 0:n], in_=Zb[0][:, 0:n])
                    I("dve", "tensor_tensor", out=osb[:, 0:n], in0=Ob[0][:, 0:n], in1=r0[:, 0:n], op=ALU.mult)
                    r1 = tmpa()
                    I("dve", "reciprocal", out=r1[:, 0:n], in_=Zb[1][:, 0:n])
                    I("dve", "scalar_tensor_tensor", out=r1[:, 0:n], in0=Ob[1][:, 0:n], scalar=nlam[:, 0:1], in1=r1[:, 0:n], op0=ALU.mult, op1=ALU.mult)
                    I("dve", "tensor_tensor", out=osb[:, 0:n], in0=osb[:, 0:n], in1=r1[:, 0:n], op=ALU.add)
                    I("act", "activation", out=osq[:, 0:n], in_=osb[:, 0:n], func=AF.Square)
                    ps = sb_()
                    I("pe", "matmul", out=ps[:, 0:n], lhsT=ones[:], rhs=osq[:, 0:n], start=True, stop=True)
                    rr = tmpa()
                    I("dve", "tensor_scalar", out=rr[:, 0:n], in0=ps[:, 0:n], scalar1=1.0 / 128.0, scalar2=1e-5, op0=ALU.mult, op1=ALU.add)
                    I("act", "activation", out=rr[:, 0:n], in_=rr[:, 0:n], func=AF.Sqrt)
                    I("dve", "reciprocal", out=rr[:, 0:n], in_=rr[:, 0:n])
                    I("dve", "scalar_tensor_tensor", out=yo[:, t0:t0 + n], in0=osb[:, 0:n], scalar=sw[:, 0:1], in1=rr[:, 0:n], op0=ALU.mult, op1=ALU.mult)
                k.dma(out=ysd.ap()[b, 1, :, hd, :], in_=yo[:])
                if hd == 0:
                    dump("att0", yo[:])
            k.barrier()

    def branch_ssd(st0, hT, li, b):
        with ExitStack() as st:
            wpool = Rot([k.sbs(st, [128, 8, 128], BF16, "wq") for _ in range(2)])
            wpool.stage = mkstage(st, 128)
            cw = k.sbs(st, [128, 6, 4], F32, "scw")
            for kk in range(4):
                k.dma(out=cw[:, :, kk], in_=W["ssm_conv_w"].ap()[li, kk].rearrange("(j p) -> p j", p=128), allow_slow_non_contiguous=True)
            cb = colvec(st, W["ssm_conv_b"].ap()[li], 6, "scb")
            nw = colvec(st, W["ssm_norm"].ap()[li], 4, "snw")
            dtb = k.sbs(st, [16, 1], F32, "dtb")
            k.dma(out=dtb[:], in_=W["ssm_dt_bias"].ap()[li].rearrange("d (h o) -> (d h) o", o=1))
            na = k.sbs(st, [16, 1], F32, "na")
            k.dma(out=na[:], in_=W["ssm_a_log"].ap()[li].rearrange("d (h o) -> (d h) o", o=1))
            I("act", "activation", out=na[:], in_=na[:], func=AF.Exp)
            I("dve", "tensor_scalar", out=na[:], in0=na[:], scalar1=-1.0, scalar2=None, op0=ALU.mult)
            dsk2 = k.sbs(st, [128, 2, 4], F32, "dsk2")
            for d in range(2):
                for hh in range(8):
                    k.dma(out=dsk2[(hh % 2) * 64:(hh % 2) * 64 + 64, d, hh // 2:hh // 2 + 1],
                          in_=W["ssm_d"].ap()[li, d, hh:hh + 1].partition_broadcast(64))
            dsk = k.sbs(st, [128, 4], F32, "dsk")
            I("dve", "tensor_tensor", out=dsk[:], in0=dsk2[:, 0, :], in1=dsk2[:, 1, :], op=ALU.add)
            esel = k.sbs(st, [16, 2048], F32, "esel")
            k.dma(out=esel[:], in_=C["k_esel"].ap())
            mask = k.sbs(st, [128, 2, 896], F32, "mask")
            k.dma(out=mask[:], in_=C["k_mask"].ap().rearrange("a p c -> p a c"))
            cT_ = k.sbs(st, [16, T], F32, "cTs")
            negc = k.sbs(st, [128, 18, 16], F32, "negc")
            dtk = k.sbs(st, [128, 18, 16], F32, "dtk")
            xsT = k.sbs(st, [128, 4, T], BF16, "xsT")
            xtok = k.sbs(st, [128, 18, 512], BF16, "xtok")
            BT = k.sbs(st, [128, T], BF16, "BT")
            CT = k.sbs(st, [128, T], BF16, "CT")
            k_zsT = k.sbs(st, [128, 4, T], BF16, "zsT")
            sA = ExitStack()
            dtT = k.sbs(sA, [16, T], F32, "dtT")
            tmp16 = k.sbs(sA, [16, T], F32, "tmp16")

            def ev_dt(ps, t0, n):
                I("act", "activation", out=dtT[:, t0:t0 + n], in_=ps[0:16, 0:n], func=AF.Identity, bias=dtb[:, 0:1])
            proj(st, hT, li, OFF["dt"], 16, ev_dt, wpool)
            I("act", "activation", out=tmp16[:], in_=dtT[:], func=AF.Abs)
            I("act", "activation", out=tmp16[:], in_=tmp16[:], func=AF.Exp, scale=-1.0)
            I("act", "activation", out=tmp16[:], in_=tmp16[:], func=AF.Ln, bias=1.0)
            I("dve", "scalar_tensor_tensor", out=dtT[:], in0=dtT[:], scalar=0.0, in1=tmp16[:], op0=ALU.max, op1=ALU.add)
            I("dve", "tensor_scalar", out=tmp16[:], in0=dtT[:], scalar1=na[:, 0:1], scalar2=None, op0=ALU.mult)
            zer = k.sbs(sA, [16, T], F32, "zer")
            I("pool", "memset", ap=zer[:], constant=1.0)
            I("dve", "tensor_tensor_scan", out=cT_[:, 0:NCTX], data0=zer[:, 0:NCTX], data1=tmp16[:, 0:NCTX], initial=0.0, op0=ALU.mult, op1=ALU.add)
            I("dve", "tensor_tensor_scan", out=cT_[:, NCTX:T], data0=zer[:, NCTX:T], data1=tmp16[:, NCTX:T], initial=cT_[:, NCTX - 1:NCTX], op0=ALU.mult, op1=ALU.add)
            crT = k.sbs(sA, [16, T], F32, "crT")
            I("dve", "tensor_tensor", out=crT[:], in0=tmp16[:], in1=cT_[:], op=ALU.subtract)
            I("dve", "tensor_scalar", out=crT[:, 0:NCTX], in0=crT[:, 0:NCTX], scalar1=cT_[:, NCTX - 1:NCTX], scalar2=None, op0=ALU.add)
            I("dve", "tensor_scalar", out=crT[:, NCTX:T], in0=crT[:, NCTX:T], scalar1=cT_[:, T - 1:T], scalar2=cT_[:, NCTX - 1:NCTX], op0=ALU.add, op1=ALU.add)
            rsel = k.sbs(sA, [16, 1], F32, "rsel")
            I("dve", "tensor_copy", out=rsel[:], in_=esel[:, 8 * 128:8 * 128 + 1])
            for j in range(9, 16):
                I("dve", "tensor_tensor", out=rsel[:], in0=rsel[:], in1=esel[:, j * 128:j * 128 + 1], op=ALU.add)
            I("dve", "tensor_tensor", out=crT[:], in0=crT[:], in1=cT_[:], op=ALU.subtract)
            I("dve", "scalar_tensor_tensor", out=cT_[:], in0=crT[:], scalar=rsel[:, 0:1], in1=cT_[:], op0=ALU.mult, op1=ALU.add)
            for tk in range(18):
                ps = PB[0]
                I("pe", "transpose", out=ps[:, 0:16], in_=cT_[0:16, tk * 128:(tk + 1) * 128], identity=ident[0:16, 0:16])
                I("pe", "transpose", out=ps[:, 16:32], in_=dtT[0:16, tk * 128:(tk + 1) * 128], identity=ident[0:16, 0:16])
                I("dve", "tensor_scalar", out=negc[:, tk, :], in0=ps[:, 0:16], scalar1=-1.0, scalar2=None, op0=ALU.mult)
                I("act", "activation", out=dtk[:, tk, :], in_=ps[:, 16:32], func=AF.Copy)
            k.barrier()
            sA.close()
            sB = ExitStack()
            xp = k.sbs(sB, [128, T + 8], F32, "sxp")
            u = k.sbs(sB, [128, T], F32, "su")
            pad_zero(xp)
            for fc in range(6):
                def ev_x(ps, t0, n):
                    pc = xp_cols(t0, n)
                    I("act", "activation", out=xp[:, pc:pc + n], in_=ps[:, 0:n], func=AF.Copy)
                proj(st, hT, li, OFF["xbc"] + fc * 128, 128, ev_x, wpool)
                conv_seg(xp, u, cw[:, fc, :], cb[:, fc:fc + 1])
                I("act", "activation", out=u[:], in_=u[:], func=AF.Silu)
                if fc < 4:
                    I("pool", "tensor_copy", out=xsT[:, fc, :], in_=u[:])
                    for tk in range(18):
                        ps = PB[1 + tk % 2]
                        I("pe", "transpose", out=ps[:, 0:128], in_=u[:, tk * 128:(tk + 1) * 128], identity=ident[:])
                        I("act", "activation", out=xtok[:, tk, fc * 128:(fc + 1) * 128], in_=ps[:, 0:128], func=AF.Copy)
                elif fc == 4:
                    I("pool", "tensor_copy", out=BT[:], in_=u[:])
                else:
                    I("pool", "tensor_copy", out=CT[:], in_=u[:])
            zsT = k_zsT
            for ch in range(4):
                def ev_z(ps, t0_, n_, ch=ch):
                    I("act", "activation", out=zsT[:, ch, t0_:t0_ + n_], in_=ps[:, 0:n_], func=AF.Silu)
                proj(st, hT, li, OFF["z"] + ch * 128, 128, ev_z, wpool)
            k.barrier()
            sB.close()
            maskneg = k.sbs(st, [128, 2, 896], F32, "maskneg")
            I("pool", "tensor_scalar", out=maskneg[:], in0=mask[:], scalar1=-1.0, scalar2=30000.0, op0=ALU.add, op1=ALU.mult)
            rowbc = [k.sbs(st, [128, 512], F32, "rowbc") for _ in range(8)]
            Lt = Rot([k.sbs(st, [128, 512], F32, "Lt") for _ in range(3)])
            Mt = Rot([k.sbs(st, [128, 512], BF16, "Mt") for _ in range(3)])
            gsb = [k.sbs(st, [128, 512], F32, "gsb") for _ in range(2)]
            gsq = k.sbs(st, [128, 512], F32, "gsq")
            zs = Rot([k.sbs(st, [128, 512], F32, "zs") for _ in range(2)])
            yo = [k.sbs(st, [128, T], BF16, "syo") for _ in range(4)]
            for (t0, n) in TT:
                lt_is_ctx = t0 < NCTX
                for g in range(2):
                    for d in range(2):
                        for hh in range(4):
                            j = d * 8 + g * 4 + hh
                            ps = PB[(d * 4 + hh) % 2]
                            I("pe", "matmul", out=ps[:, 0:n], lhsT=esel[:, j * 128:(j + 1) * 128], rhs=cT_[:, t0:t0 + n], start=True, stop=True)
                            I("act", "activation", out=rowbc[d * 4 + hh][:, 0:n], in_=ps[:, 0:n], func=AF.Copy)
                    ybank = [PB[2], PB[3]]
                    contrib = []
                    for sc in range(18):
                        s_is_ctx = sc < 2
                        for d in range(2):
                            if lt_is_ctx:
                                if not s_is_ctx:
                                    continue
                                contrib.append((sc, d, "diag", sc))
                            else:
                                if s_is_ctx:
                                    contrib.append((sc, d, "full", 0))
                                    continue
                                i_ = sc - 2
                                jt = (t0 - NCTX) // 512
                                if 4 * jt <= i_ <= 4 * jt + 3:
                                    contrib.append((sc, d, "diag", i_ - 4 * jt))
                                elif (d == 0 and i_ < 4 * jt) or (d == 1 and i_ > 4 * jt + 3):
                                    contrib.append((sc, d, "full", 0))
                    first = {hh: True for hh in range(4)}
                    last_idx = len(contrib) - 1
                    cbb = Rot(PB[4:6])
                    cur_sc = None
                    cbps = None
                    for ci, (sc, d, kind, rel) in enumerate(contrib):
                        if sc != cur_sc:
                            cur_sc = sc
                            cbps = cbb()
                            I("pe", "matmul", out=cbps[:, 0:n], lhsT=BT[g * 64:(g + 1) * 64, sc * 128:(sc + 1) * 128],
                              rhs=CT[g * 64:(g + 1) * 64, t0:t0 + n], start=True, stop=True)
                        for hh in range(4):
                            hidx = g * 4 + hh
                            j = d * 8 + hidx
                            rb = rowbc[d * 4 + hh]
                            L_ = Lt()
                            if kind == "full":
                                I("act", "activation", out=L_[:, 0:n], in_=rb[:, 0:n], func=AF.Exp, bias=negc[:, sc, j:j + 1])
                            else:
                                mo = 384 - rel * 128
                                I("dve", "scalar_tensor_tensor", out=L_[:, 0:n], in0=rb[:, 0:n], scalar=negc[:, sc, j:j + 1], in1=maskneg[:, d, mo:mo + n], op0=ALU.add, op1=ALU.add)
                                I("act", "activation", out=L_[:, 0:n], in_=L_[:, 0:n], func=AF.Exp)
                            M_ = Mt()
                            I("dve", "scalar_tensor_tensor", out=M_[:, 0:n], in0=L_[:, 0:n], scalar=dtk[:, sc, j:j + 1], in1=cbps[:, 0:n], op0=ALU.mult, op1=ALU.mult)
                            is_last = (ci == last_idx)
                            yb = ybank[hh // 2]
                            I("pe", "matmul", out=yb[(hh % 2) * 64:(hh % 2) * 64 + 64, 0:n], lhsT=xtok[:, sc, hidx * 64:(hidx + 1) * 64],
                              rhs=M_[:, 0:n], start=first[hh], stop=is_last)
                            first[hh] = False
                    for cc in range(2):
                        ch = g * 2 + cc
                        gs = gsb[cc]
                        I("dve", "scalar_tensor_tensor", out=gs[:, 0:n], in0=xsT[:, ch, t0:t0 + n], scalar=dsk[:, ch:ch + 1], in1=ybank[cc][:, 0:n], op0=ALU.mult, op1=ALU.add)
                    for cc in range(2):
                        ch = g * 2 + cc
                        I("dve", "tensor_tensor", out=gsb[cc][:, 0:n], in0=gsb[cc][:, 0:n], in1=zsT[:, ch, t0:t0 + n], op=ALU.mult)
                    ps = PB[6]
                    for cc in range(2):
                        I("act", "activation", out=gsq[:, 0:n], in_=gsb[cc][:, 0:n], func=AF.Square)
                        I("pe", "matmul", out=ps[:, 0:n], lhsT=ones[:], rhs=gsq[:, 0:n], start=(cc == 0), stop=(cc == 1))
                    rr = zs()
                    I("dve", "tensor_scalar", out=rr[:, 0:n], in0=ps[:, 0:n], scalar1=1.0 / 256.0, scalar2=1e-5, op0=ALU.mult, op1=ALU.add)
                    I("act", "activation", out=rr[:, 0:n], in_=rr[:, 0:n], func=AF.Sqrt)
                    I("dve", "reciprocal", out=rr[:, 0:n], in_=rr[:, 0:n])
                    for cc in range(2):
                        ch = g * 2 + cc
                        I("dve", "scalar_tensor_tensor", out=yo[ch][:, t0:t0 + n], in0=gsb[cc][:, 0:n], scalar=nw[:, ch:ch + 1], in1=rr[:, 0:n], op0=ALU.mult, op1=ALU.mult)
            for ch in range(4):
                k.dma(out=ysd.ap()[b, 3, :, ch, :], in_=yo[ch][:])
            dump("ssm0", yo[0][:])
            k.barrier()

    def bcast_row(st, src1d, n, name="bc"):
        t = k.sbs(st, [128, n], F32, name)
        k.dma(out=t[:], in_=src1d.partition_broadcast(128))
        return t

    def layer_norm_rows(xt, tmp_pool, gbc, bbc, eps):
        stats, mv, rs, xn = tmp_pool
        for h in range(2):
            I("dve", "bn_stats", out=stats[:, h, :], in_=xt[:, h * 512:(h + 1) * 512])
        I("dve", "bn_aggr", out=mv[:], in_=stats[:])
        rstd_of(mv[:, 1:2], eps, rs[:])
        I("dve", "tensor_scalar", out=xt[:], in0=xt[:], scalar1=mv[:, 0:1], scalar2=rs[:, 0:1], op0=ALU.subtract, op1=ALU.mult)
        I("pool", "tensor_tensor", out=xt[:], in0=xt[:], in1=gbc[:], op=ALU.mult)
        I("pool", "tensor_tensor", out=xt[:], in0=xt[:], in1=bbc[:], op=ALU.add)

    def mixer(li, b):
        with ExitStack() as st:
            modc, modc1p = load_modc(st, li)
            hT = k.sbs(st, [128, 8, T], BF16, "hT")
            with ExitStack() as s2:
                tmp_pool = (k.sbs(s2, [128, 2, 6], F32, "stats"), k.sbs(s2, [128, 2], F32, "mv"), k.sbs(s2, [128, 1], F32, "rs"), k.sbs(s2, [128, D], F32, "xn"))
                xts = Rot([k.sbs(s2, [128, D], F32, "xt") for _ in range(2)])
                for tk in range(18):
                    xt = xts()
                    k.dma(out=xt[:], in_=xres.ap()[b, tk * 128:(tk + 1) * 128, :])
                    r = 2 if tk < 2 else b
                    ln_mod_T(s2, xt, hT, tk * 128, modc, modc1p, r, 8, 0, tmp_pool)
                k.barrier()
            if b == 0 and li == 0:
                dump("hT", hT[:, 0, :])
            if "rglru" in stages:
                branch_rglru(st, hT, li, b)
            if "pool" in stages:
                branch_pool(st, hT, li, b)
            if "attn" in stages:
                branch_attn(st, hT, li, b)
            if "ssd" in stages:
                branch_ssd(st, hT, li, b)
            if "merge" not in stages:
                return
            with ExitStack() as s2:
              mT = k.sbs(s2, [128, 8, T], BF16, "mT")
              with ExitStack() as s3:
                ysT = [k.sbs(s3, [128, 4, T], BF16, "ysT") for _ in range(4)]
                for i in range(4):
                    k.dma(out=ysT[i][:], in_=ysd.ap()[b, i])
                wbr = Rot([k.sbs(s3, [128, 4, 4, 128], BF16, "wbr") for _ in range(2)])
                wmg = Rot([k.sbs(s3, [128, 8, 4, 128], BF16, "wmg") for _ in range(2)])
                gs = Rot([k.sbs(s3, [128, 512], F32, "gs") for _ in range(3)])
                mstage = mkstage(s3, 128, nbuf=2)
                acc = Rot([k.sbs(s3, [128, 512], F32, "macc") for _ in range(2)])
                pbk = Rot(PB[0:4])
                gbk = Rot(PB[4:8])
                for dc in range(8):
                    wb_ = wbr()
                    wm_ = wmg()
                    for i in range(4):
                        wload(wb_[:, :, i, :], W["w_branch"].ap()[li, i][:, dc * 128:(dc + 1) * 128].rearrange("(j p) n -> p j n", p=128), mstage)
                        wload(wm_[:, :, i, :], wview("w_in", li, OFF["mg"] + i * 1024 + dc * 128, 128), mstage)
                    for (t0, n) in TT:
                        a_ = acc()
                        for i in range(4):
                            pp = pbk()
                            for j in range(4):
                                I("pe", "matmul", out=pp[:, 0:n], lhsT=wb_[:, j, i, :], rhs=ysT[i][:, j, t0:t0 + n], start=(j == 0), stop=(j == 3))
                            pg = gbk()
                            for j in range(8):
                                I("pe", "matmul", out=pg[:, 0:n], lhsT=wm_[:, j, i, :], rhs=hT[:, j, t0:t0 + n], start=(j == 0), stop=(j == 7))
                            g_ = gs()
                            I("act", "activation", out=g_[:, 0:n], in_=pg[:, 0:n], func=AF.Sigmoid)
                            if i == 0:
                                I("dve", "tensor_tensor", out=a_[:, 0:n], in0=pp[:, 0:n], in1=g_[:, 0:n], op=ALU.mult)
                            else:
                                I("dve", "tensor_tensor", out=g_[:, 0:n], in0=pp[:, 0:n], in1=g_[:, 0:n], op=ALU.mult)
                                if i < 3:
                                    I("pool", "tensor_tensor", out=a_[:, 0:n], in0=a_[:, 0:n], in1=g_[:, 0:n], op=ALU.add)
                                else:
                                    I("pool", "tensor_tensor", out=mT[:, dc, t0:t0 + n], in0=a_[:, 0:n], in1=g_[:, 0:n], op=ALU.add)
                if b == 0 and li == 0:
                    dump("mT", mT[:, 0, :])
                k.barrier()
              if True:
                wout = k.sbs(s2, [128, 8, D], BF16, "wout")
                ostage = mkstage(s2, 256, nbuf=2)
                for hh in range(4):
                    wload(wout[:, :, hh * 256:(hh + 1) * 256], W["w_out"].ap()[li][:, hh * 256:(hh + 1) * 256].rearrange("(j p) n -> p j n", p=128), ostage)
                g1l = bcast_row(s2, modrow.ap()[li, b, 2048:3072], D, "g1l")
                g1c = bcast_row(s2, modrow.ap()[li, 2, 2048:3072], D, "g1c")
                lg = bcast_row(s2, W["ln1_g"].ap()[li], D, "lg")
                lb = bcast_row(s2, W["ln1_b"].ap()[li], D, "lb")
                tmp_pool = (k.sbs(s2, [128, 2, 6], F32, "stats"), k.sbs(s2, [128, 2], F32, "mv"), k.sbs(s2, [128, 1], F32, "rs"), None)
                xts = Rot([k.sbs(s2, [128, D], F32, "xt") for _ in range(2)])
                yts = Rot([k.sbs(s2, [128, D], F32, "yt") for _ in range(2)])
                ob = Rot(PB[0:4])
                for tk in range(18):
                    xt = xts()
                    yt = yts()
                    k.dma(out=xt[:], in_=xres.ap()[b, tk * 128:(tk + 1) * 128, :])
                    gb = g1c if tk < 2 else g1l
                    for hh in range(2):
                        ps = ob()
                        for j in range(8):
                            I("pe", "matmul", out=ps[:], lhsT=mT[:, j, tk * 128:(tk + 1) * 128], rhs=wout[:, j, hh * 512:(hh + 1) * 512], start=(j == 0), stop=(j == 7))
                        I("dve", "tensor_tensor", out=yt[:, hh * 512:(hh + 1) * 512], in0=ps[:], in1=gb[:, hh * 512:(hh + 1) * 512], op=ALU.mult)
                    I("dve", "scalar_tensor_tensor", out=xt[:], in0=xt[:], scalar=ALPHA, in1=yt[:], op0=ALU.mult, op1=ALU.add)
                    layer_norm_rows(xt, tmp_pool, lg, lb, 1e-5)
                    k.dma(out=xres.ap()[b, tk * 128:(tk + 1) * 128, :], in_=xt[:])
                    if b == 0 and li == 0 and tk == 2:
                        dump("x1", xt[:])
                k.barrier()
            k.barrier()

    def moe(li, b, last):
        with ExitStack() as st:
            modc, modc1p = load_modc(st, li)
            h2T = k.sbs(st, [128, 8, T], BF16, "h2T")
            G = k.sbs(st, [128, 18, NEXP], F32, "G")
            acc = k.sbs(st, [128, 18, D], F32, "acc")
            with ExitStack() as s2:
                tmp_pool = (k.sbs(s2, [128, 2, 6], F32, "stats"), k.sbs(s2, [128, 2], F32, "mv"), k.sbs(s2, [128, 1], F32, "rs"), k.sbs(s2, [128, D], F32, "xn"))
                xts = Rot([k.sbs(s2, [128, D], F32, "xt") for _ in range(2)])
                h2f = k.sbs(s2, [128, 8, 128], F32, "h2f")
                rw = k.sbs(s2, [128, 8, NEXP], F32, "rw")
                k.dma(out=rw[:], in_=W["router_w"].ap()[li].rearrange("(j p) n -> p j n", p=128))
                rb = bcast_row(s2, W["router_b"].ap()[li], NEXP, "rb")
                bd = k.sbs(s2, [NEXP, D], F32, "bd")
                k.dma(out=bd[:], in_=W["b_down"].ap()[li])
                lg_ = k.sbs(s2, [128, NEXP], F32, "lg_")
                m8 = k.sbs(s2, [128, 8], F32, "m8")
                nm = k.sbs(s2, [128, 1], F32, "nm")
                ex = k.sbs(s2, [128, NEXP], F32, "ex")
                mk = k.sbs(s2, [128, NEXP], F32, "mk")
                ssum = k.sbs(s2, [128, 1], F32, "ssum")
                GT = k.sbs(s2, [NEXP, 128], F32, "GT")
                for tk in range(18):
                    xt = xts()
                    k.dma(out=xt[:], in_=xres.ap()[b, tk * 128:(tk + 1) * 128, :])
                    r = 2 if tk < 2 else b
                    ln_mod_T(s2, xt, h2T, tk * 128, modc, modc1p, r, 32, 24, tmp_pool, extra_f32=h2f)
                    ps = PB[0]
                    for j in range(8):
                        I("pe", "matmul", out=ps[:, 0:NEXP], lhsT=h2f[:, j, :], rhs=rw[:, j, :], start=(j == 0), stop=(j == 7))
                    I("dve", "tensor_tensor", out=lg_[:], in0=ps[:, 0:NEXP], in1=rb[:], op=ALU.add)
                    I("dve", "max", out=m8[:], in_=lg_[:])
                    I("dve", "tensor_scalar", out=mk[:], in0=lg_[:], scalar1=m8[:, 3:4], scalar2=None, op0=ALU.is_ge)
                    I("dve", "tensor_scalar", out=nm[:], in0=m8[:, 0:1], scalar1=-1.0, scalar2=None, op0=ALU.mult)
                    I("act", "activation", out=ex[:], in_=lg_[:], func=AF.Exp, bias=nm[:, 0:1])
                    I("dve", "tensor_tensor", out=ex[:], in0=ex[:], in1=mk[:], op=ALU.mult)
                    I("dve", "reduce_sum", out=ssum[:], in_=ex[:], axis=AX.X)
                    I("dve", "reciprocal", out=ssum[:], in_=ssum[:])
                    I("dve", "tensor_scalar", out=G[:, tk, :], in0=ex[:], scalar1=ssum[:, 0:1], scalar2=None, op0=ALU.mult)
                    ps2 = PB[1]
                    I("pe", "transpose", out=ps2[0:NEXP, 0:128], in_=G[:, tk, :], identity=ident[:])
                    I("act", "activation", out=GT[:], in_=ps2[0:NEXP, 0:128], func=AF.Copy)
                    for hh in range(2):
                        ps3 = PB[2 + hh]
                        I("pe", "matmul", out=ps3[:], lhsT=GT[:], rhs=bd[:, hh * 512:(hh + 1) * 512], start=True, stop=True)
                        I("act", "activation", out=acc[:, tk, hh * 512:(hh + 1) * 512], in_=ps3[:], func=AF.Copy)
                if b == 0 and li == 0:
                    dump("G", G[:, 2, :])
                k.barrier()
            with ExitStack() as s2:
                actT = k.sbs(s2, [128, 8, T], BF16, "actT")
                bgc = k.sbs(s2, [128, 16, NEXP], F32, "bgc")
                bg17 = k.sbs(s2, [128, 8, NEXP], F32, "bg17")
                bu1 = k.sbs(s2, [128, 8, NEXP], F32, "bu1")
                sG = ExitStack()
                bgr = k.sbs(sG, [NEXP, 2048], F32, "bgr")
                k.dma(out=bgr[:], in_=W["b_gate_up"].ap()[li])
                for ch in range(16):
                    ps = PB[ch % 2]
                    I("pe", "transpose", out=ps[:, 0:NEXP], in_=bgr[0:NEXP, ch * 128:(ch + 1) * 128], identity=ident[0:NEXP, 0:NEXP])
                    I("act", "activation", out=bgc[:, ch, :], in_=ps[:, 0:NEXP], func=AF.Copy)
                I("dve", "tensor_scalar", out=bg17[:], in0=bgc[:, 0:8, :], scalar1=1.702, scalar2=None, op0=ALU.mult)
                I("dve", "tensor_scalar", out=bu1[:], in0=bgc[:, 8:16, :], scalar1=1.0, scalar2=None, op0=ALU.add)
                k.barrier()
                sG.close()
                stg = Rot([k.sbs(s2, [128, 8, 256], F32, "stg") for _ in range(3)])
                wbf = Rot([k.sbs(s2, [128, 8, 256], BF16, "wbf") for _ in range(4)])
                TTm = TT[1:] if last else TT
                tks = list(range(2, 18)) if last else list(range(18))
                gt_ = Rot([k.sbs(s2, [128, 512], F32, "gt") for _ in range(2)])
                sg_ = Rot([k.sbs(s2, [128, 512], F32, "sg") for _ in range(2)])
                ut_ = Rot([k.sbs(s2, [128, 512], F32, "ut") for _ in range(2)])
                gb = Rot(PB[0:3])
                ub = Rot(PB[3:6])
                db = Rot(PB[6:8])
                SIGC = 1.0 / (1.0 + math.exp(-1.702 * 7.0))
                pieces = []
                for e in range(NEXP):
                    for jp in range(8):
                        pieces.append(("gu", e, jp))
                    for q4 in range(4):
                        pieces.append(("dn", e, q4))

                def load_piece(pc):
                    kind, e, idx = pc
                    s_ = stg()
                    w_ = wbf()
                    if kind == "gu":
                        k.dma(out=s_[:, :, 0:128], in_=W["w_gate_up"].ap()[li, e][:, idx * 128:(idx + 1) * 128].rearrange("(j p) n -> p j n", p=128))
                        k.dma(out=s_[:, :, 128:256], in_=W["w_gate_up"].ap()[li, e][:, D + idx * 128:D + (idx + 1) * 128].rearrange("(j p) n -> p j n", p=128))
                    else:
                        k.dma(out=s_[:], in_=W["w_down"].ap()[li, e][:, idx * 256:(idx + 1) * 256].rearrange("(j p) n -> p j n", p=128))
                    I("pool", "tensor_copy", out=w_[:], in_=s_[:])
                    return w_

                PF = 2
                loaded = {}
                for i in range(min(PF, len(pieces))):
                    loaded[i] = load_piece(pieces[i])
                for pi, (kind, e, idx) in enumerate(pieces):
                    if pi + PF < len(pieces):
                        loaded[pi + PF] = load_piece(pieces[pi + PF])
                    w_ = loaded.pop(pi)
                    if kind == "gu":
                        jp = idx
                        for (t0, n) in TTm:
                            pg = gb()
                            for j in range(8):
                                I("pe", "matmul", out=pg[:, 0:n], lhsT=w_[:, j, 0:128], rhs=h2T[:, j, t0:t0 + n], start=(j == 0), stop=(j == 7))
                            pu = ub()
                            for j in range(8):
                                I("pe", "matmul", out=pu[:, 0:n], lhsT=w_[:, j, 128:256], rhs=h2T[:, j, t0:t0 + n], start=(j == 0), stop=(j == 7))
                            gt = gt_()
                            sg = sg_()
                            ut = ut_()
                            I("act", "activation", out=sg[:, 0:n], in_=pg[:, 0:n], func=AF.Sigmoid, scale=1.702, bias=bg17[:, jp, e:e + 1])
                            I("dve", "tensor_scalar", out=gt[:, 0:n], in0=pg[:, 0:n], scalar1=bgc[:, jp, e:e + 1], scalar2=7.0, op0=ALU.add, op1=ALU.min)
                            I("dve", "tensor_scalar", out=ut[:, 0:n], in0=pu[:, 0:n], scalar1=bu1[:, jp, e:e + 1], scalar2=8.0, op0=ALU.add, op1=ALU.min)
                            I("dve", "scalar_tensor_tensor", out=gt[:, 0:n], in0=sg[:, 0:n], scalar=SIGC, in1=gt[:, 0:n], op0=ALU.min, op1=ALU.mult)
                            I("dve", "scalar_tensor_tensor", out=actT[:, jp, t0:t0 + n], in0=ut[:, 0:n], scalar=-6.0, in1=gt[:, 0:n], op0=ALU.max, op1=ALU.mult)
                    else:
                        q4 = idx
                        for tk in tks:
                            ps = db()
                            for j in range(8):
                                I("pe", "matmul", out=ps[:, 0:256], lhsT=actT[:, j, tk * 128:(tk + 1) * 128], rhs=w_[:, j, :], start=(j == 0), stop=(j == 7))
                            I("dve", "scalar_tensor_tensor", out=acc[:, tk, q4 * 256:(q4 + 1) * 256], in0=ps[:, 0:256], scalar=G[:, tk, e:e + 1],
                              in1=acc[:, tk, q4 * 256:(q4 + 1) * 256], op0=ALU.mult, op1=ALU.add)
                print("moe sbuf remaining", nc.sbuf_bytes_remaining, flush=True)
                k.barrier()
            if b == 0 and li == 0:
                dump("moe", acc[:, 2, :])
            with ExitStack() as s2:
                g2l = bcast_row(s2, modrow.ap()[li, b, 5120:6144], D, "g2l")
                g2c = bcast_row(s2, modrow.ap()[li, 2, 5120:6144], D, "g2c")
                lg = bcast_row(s2, W["ln2_g"].ap()[li], D, "lg2")
                lb = bcast_row(s2, W["ln2_b"].ap()[li], D, "lb2")
                tmp_pool = (k.sbs(s2, [128, 2, 6], F32, "stats"), k.sbs(s2, [128, 2], F32, "mv"), k.sbs(s2, [128, 1], F32, "rs"), None)
                xts = Rot([k.sbs(s2, [128, D], F32, "xt") for _ in range(2)])
                for tk in range(18):
                    if last and tk < 2:
                        continue
                    xt = xts()
                    k.dma(out=xt[:], in_=xres.ap()[b, tk * 128:(tk + 1) * 128, :])
                    gb_ = g2c if tk < 2 else g2l
                    I("pool", "tensor_tensor", out=acc[:, tk, :], in0=acc[:, tk, :], in1=gb_[:], op=ALU.mult)
                    I("dve", "scalar_tensor_tensor", out=xt[:], in0=xt[:], scalar=ALPHA, in1=acc[:, tk, :], op0=ALU.mult, op1=ALU.add)
                    layer_norm_rows(xt, tmp_pool, lg, lb, 1e-5)
                    if last:
                        k.dma(out=out.ap()[b, (tk - 2) * 128:(tk - 1) * 128, :], in_=xt[:])
                    else:
                        k.dma(out=xres.ap()[b, tk * 128:(tk + 1) * 128, :], in_=xt[:])
                k.barrier()
            k.barrier()

    for li in range(depth):
        adaln(li)
        for b in range(nb):
            mixer(li, b)
            if stop_after == "mixer":
                continue
            moe(li, b, last=(li == depth - 1 and stop_after is None))
    k.finish([out] + list(DBG.values()))
    print("instructions", k.n_inst, "waits", k.n_wait, flush=True)
    return nc


_NC_CACHE = {}


def kernel(**inputs):
    inputs = {k_: np.ascontiguousarray(np.asarray(v), dtype=np.float32) for k_, v in inputs.items()}
    if "nc" not in _NC_CACHE:
        _NC_CACHE["nc"] = build()
    nc = _NC_CACHE["nc"]
    consts = host_consts()
    in_maps = []
    for core in range(8):
        m = {}
        for name, arr in inputs.items():
            if name in ("x", "c", "ctx"):
                m[name] = np.ascontiguousarray(arr[2 * core:2 * core + 2])
            else:
                m[name] = arr
        m.update(consts)
        in_maps.append(m)
    res = run_bass_kernel_spmd(nc, in_maps, core_ids=list(range(8)))
    return np.concatenate([r["out"] for r in res.results], axis=0).astype(np.float32)
```
